# Optimizing a Trainium2 kernel written in Bass

```python
import math
import jax, jax.numpy as jnp
from jax import lax
import numpy as np

D_MODEL = 1024
BATCH = 4
SEQ = 4096
DEPTH = 1

MIX_WIDTH = D_MODEL
GLA_WIDTH = MIX_WIDTH // 2
GLA_HEADS = 4
GLA_DV = GLA_WIDTH // GLA_HEADS
GLA_DK = GLA_DV // 2
GLA_KEY_WIDTH = GLA_HEADS * GLA_DK
GLA_GATE_RANK = 16
GLA_GATE_TEMP = 16.0
GLA_CHUNK = 16
DIL_WIDTH = MIX_WIDTH - GLA_WIDTH
DIL_HEADS = 8
DIL_DH = DIL_WIDTH // DIL_HEADS
DIL_CONFIGS = ((128, 1), (512, 4), (2048, 16))
DIL_BLOCK = 128
N_GROUPS = 4
EXPERTS_PER_GROUP = 8
N_EXPERTS = N_GROUPS * EXPERTS_PER_GROUP
TOP_K_INNER = 2
D_FF_EXPERT = 512
MOE_BLOCK = 128
DEEPNORM_ALPHA = (2.0 * DEPTH) ** 0.25
DEEPNORM_BETA = (8.0 * DEPTH) ** -0.25
EPS = 1e-5
SPLIT_SIZES = (GLA_KEY_WIDTH, GLA_KEY_WIDTH, GLA_WIDTH, GLA_WIDTH, GLA_GATE_RANK, DIL_WIDTH, DIL_WIDTH, DIL_WIDTH)
SPLIT_IS_VALUE = (False, False, True, False, False, False, False, True)
SPLIT_POINTS = tuple(int(v) for v in np.cumsum(SPLIT_SIZES)[:-1])
PROJ_WIDTH = int(sum(SPLIT_SIZES))

kernel_name = "hymba_gla_dilated_alibi_hmoe_deepnorm"


def layer_norm(x, g, b):
    xf = x.astype(jnp.float32)
    mu = jnp.mean(xf, axis=-1, keepdims=True)
    var = jnp.mean(jnp.square(xf - mu), axis=-1, keepdims=True)
    return ((xf - mu) * lax.rsqrt(var + EPS)).astype(x.dtype) * g + b


def rms_norm(x, g):
    xf = x.astype(jnp.float32)
    return (xf * lax.rsqrt(jnp.mean(xf * xf, axis=-1, keepdims=True) + EPS)).astype(x.dtype) * g


def to_heads(t, n_heads):
    b, s, w = t.shape
    return t.reshape(b, s, n_heads, w // n_heads).transpose(0, 2, 1, 3)


def from_heads(t):
    b, h, s, d = t.shape
    return t.transpose(0, 2, 1, 3).reshape(b, s, h * d)


def gla_chunked(q, k, v, log_a):
    B, H, S, dk = q.shape
    dv = v.shape[-1]
    C = GLA_CHUNK
    n = S // C
    q = q.reshape(B, H, n, C, dk)
    k = k.reshape(B, H, n, C, dk)
    v = v.reshape(B, H, n, C, dv)
    b = jnp.cumsum(log_a.astype(jnp.float32).reshape(B, H, n, C, dk), axis=3)
    b_last = b[:, :, :, -1:, :]
    causal = jnp.tril(jnp.ones((C, C), dtype=bool))
    diff = b[:, :, :, :, None, :] - b[:, :, :, None, :, :]
    decay_ij = jnp.exp(jnp.where(causal[:, :, None], diff, -jnp.inf))
    A = jnp.sum(q[:, :, :, :, None, :] * k[:, :, :, None, :, :] * decay_ij, axis=-1)
    o_intra = jnp.einsum('bhnij,bhnje->bhnie', A, v)
    dS = jnp.einsum('bhncd,bhnce->bhnde', k * jnp.exp(b_last - b), v)
    chunk_decay = jnp.exp(b_last[:, :, :, 0, :])

    def step(state, inp):
        ds_n, dec_n = inp
        return state * dec_n[..., None] + ds_n, state

    s0 = jnp.zeros((B, H, dk, dv), dS.dtype)
    _, s_before = lax.scan(step, s0, (jnp.moveaxis(dS, 2, 0), jnp.moveaxis(chunk_decay, 2, 0)))
    s_before = jnp.moveaxis(s_before, 0, 2)
    o_inter = jnp.einsum('bhncd,bhnde->bhnce', q * jnp.exp(b), s_before)
    return (o_intra + o_inter).reshape(B, H, S, dv).astype(v.dtype)


def dilated_branch(q, k, v, slopes, window, dilation):
    B, H, S, dh = q.shape
    r = dilation
    L = S // r
    w = window // r
    nb = -(-L // DIL_BLOCK)
    Lp = nb * DIL_BLOCK

    def to_sub(t):
        t = t.reshape(B, H, L, r, dh).transpose(0, 1, 3, 2, 4)
        t = jnp.pad(t, ((0, 0), (0, 0), (0, 0), (0, Lp - L), (0, 0)))
        return t.reshape(B, H, r, nb, DIL_BLOCK, dh)

    qs, ks, vs = to_sub(q), to_sub(k), to_sub(v)

    def band(t):
        tp = jnp.concatenate([jnp.zeros_like(t[:, :, :, :1]), t], axis=3)
        return jnp.concatenate([tp[:, :, :, :-1], tp[:, :, :, 1:]], axis=4)

    kb, vb = band(ks), band(vs)
    scores = jnp.einsum('bhrnid,bhrnjd->bhrnij', qs, kb).astype(jnp.float32) * (DIL_DH ** -0.5)
    i_loc = jnp.arange(DIL_BLOCK)[:, None]
    j_loc = jnp.arange(2 * DIL_BLOCK)[None, :]
    rel = i_loc + DIL_BLOCK - j_loc
    blk = jnp.arange(nb)[:, None, None]
    valid = (rel >= 0) & (rel <= w) & (blk * DIL_BLOCK + j_loc - DIL_BLOCK >= 0)
    alibi = -slopes[:, None, None] * (rel * r).astype(jnp.float32)[None]
    scores = jnp.where(valid[None, None, None], scores + alibi[None, :, None, None], -jnp.inf)
    m = jnp.max(scores, axis=-1, keepdims=True)
    p = jnp.exp(scores - m)
    z = jnp.sum(p, axis=-1, keepdims=True)
    out = jnp.einsum('bhrnij,bhrnjd->bhrnid', (p / z).astype(v.dtype), vb)
    lse = (m + jnp.log(z))[..., 0]
    out = out.reshape(B, H, r, Lp, dh)[:, :, :, :L].transpose(0, 1, 3, 2, 4).reshape(B, H, S, dh)
    lse = lse.reshape(B, H, r, Lp)[:, :, :, :L].transpose(0, 1, 3, 2).reshape(B, H, S)
    return out, lse


def dilated_mixture(q, k, v, slopes):
    outs, lses = [], []
    for window, dilation in DIL_CONFIGS:
        o, lse = dilated_branch(q, k, v, slopes, window, dilation)
        outs.append(o)
        lses.append(lse)
    wts = jax.nn.softmax(jnp.stack(lses, axis=0), axis=0)
    return jnp.einsum('cbhs,cbhsd->bhsd', wts.astype(q.dtype), jnp.stack(outs, axis=0))


def grouped_expert_ffn(ht, expert_id, gate, w_gate, w_up, w_down):
    T, D = ht.shape
    K = expert_id.shape[1]
    P = T * K
    flat_e = expert_id.reshape(P)
    flat_tok = jnp.arange(P, dtype=jnp.int32) // K
    order = jnp.argsort(flat_e)
    e_sorted = flat_e[order]
    counts = jnp.bincount(flat_e, length=N_EXPERTS)
    padded = ((counts + MOE_BLOCK - 1) // MOE_BLOCK) * MOE_BLOCK
    start_sorted = jnp.cumsum(counts) - counts
    end_padded = jnp.cumsum(padded)
    start_padded = end_padded - padded
    dest = start_padded[e_sorted] + (jnp.arange(P) - start_sorted[e_sorted])
    n_rows = P + N_EXPERTS * MOE_BLOCK
    n_blocks = n_rows // MOE_BLOCK
    buf = jnp.zeros((n_rows, D), ht.dtype).at[dest].set(ht[flat_tok[order]])
    block_expert = jnp.minimum(
        jnp.searchsorted(end_padded, jnp.arange(n_blocks) * MOE_BLOCK, side='right'), N_EXPERTS - 1)

    def block_ffn(args):
        xblk, e = args
        hid = jax.nn.silu(xblk @ w_gate[e]) * (xblk @ w_up[e])
        return hid @ w_down[e]

    yb = lax.map(block_ffn, (buf.reshape(n_blocks, MOE_BLOCK, D), block_expert))
    y_sorted = yb.reshape(n_rows, D)[dest]
    y_pairs = jnp.zeros((P, D), yb.dtype).at[order].set(y_sorted).reshape(T, K, D)
    return jnp.einsum('tkd,tk->td', y_pairs, gate.astype(y_pairs.dtype))


def hier_moe(ht, rc_w, rc_b, rf_w, rf_b, w_gate, w_up, w_down):
    coarse = (ht @ rc_w + rc_b).astype(jnp.float32)
    p_coarse = jax.nn.softmax(coarse, axis=-1)
    g_idx = jnp.argmax(coarse, axis=-1).astype(jnp.int32)
    p_group = jnp.take_along_axis(p_coarse, g_idx[:, None], axis=1)
    fine = (jnp.einsum('td,dge->tge', ht, rf_w) + rf_b).astype(jnp.float32)
    fine_sel = jnp.take_along_axis(fine, g_idx[:, None, None], axis=1)[:, 0]
    top_val, top_idx = lax.top_k(fine_sel, TOP_K_INNER)
    gate = p_group * jax.nn.softmax(top_val, axis=-1)
    expert_id = g_idx[:, None] * EXPERTS_PER_GROUP + top_idx.astype(jnp.int32)
    return grouped_expert_ffn(ht, expert_id, gate, w_gate, w_up, w_down)


def setup_inputs(seed: int = 0) -> dict:
    key = jax.random.key(seed)
    ks = jax.random.split(key, 20)
    f32 = jnp.float32

    def nrm(k, shape, scale):
        return jax.random.normal(k, shape, f32) * scale

    col_scale = jnp.asarray(np.concatenate(
        [np.full((sz,), DEEPNORM_BETA if is_v else 1.0, np.float32) for sz, is_v in zip(SPLIT_SIZES, SPLIT_IS_VALUE)]))
    L = DEPTH
    return {
        "x": nrm(ks[0], (BATCH, SEQ, D_MODEL), 1.0),
        "w_in": nrm(ks[1], (L, D_MODEL, PROJ_WIDTH), D_MODEL ** -0.5) * col_scale,
        "gla_gate_w2": nrm(ks[2], (L, GLA_GATE_RANK, GLA_KEY_WIDTH), GLA_GATE_RANK ** -0.5),
        "gla_gate_b": nrm(ks[3], (L, GLA_KEY_WIDTH), 0.1),
        "gla_norm_g": 1.0 + nrm(ks[4], (L, GLA_DV), 0.02),
        "dil_norm_g": 1.0 + nrm(ks[5], (L, DIL_DH), 0.02),
        "w_out": nrm(ks[6], (L, MIX_WIDTH, D_MODEL), MIX_WIDTH ** -0.5 * DEEPNORM_BETA),
        "ln1_g": 1.0 + nrm(ks[7], (L, D_MODEL), 0.02),
        "ln1_b": nrm(ks[8], (L, D_MODEL), 0.02),
        "router_coarse_w": nrm(ks[9], (L, D_MODEL, N_GROUPS), D_MODEL ** -0.5),
        "router_coarse_b": nrm(ks[10], (L, N_GROUPS), 0.01),
        "router_fine_w": nrm(ks[11], (L, D_MODEL, N_GROUPS, EXPERTS_PER_GROUP), D_MODEL ** -0.5),
        "router_fine_b": nrm(ks[12], (L, N_GROUPS, EXPERTS_PER_GROUP), 0.01),
        "expert_w_gate": nrm(ks[13], (L, N_EXPERTS, D_MODEL, D_FF_EXPERT), D_MODEL ** -0.5),
        "expert_w_up": nrm(ks[14], (L, N_EXPERTS, D_MODEL, D_FF_EXPERT), D_MODEL ** -0.5 * DEEPNORM_BETA),
        "expert_w_down": nrm(ks[15], (L, N_EXPERTS, D_FF_EXPERT, D_MODEL), D_FF_EXPERT ** -0.5 * DEEPNORM_BETA),
        "ln2_g": 1.0 + nrm(ks[16], (L, D_MODEL), 0.02),
        "ln2_b": nrm(ks[17], (L, D_MODEL), 0.02),
    }


def reference(x, w_in, gla_gate_w2, gla_gate_b, gla_norm_g, dil_norm_g, w_out, ln1_g, ln1_b,
              router_coarse_w, router_coarse_b, router_fine_w, router_fine_b,
              expert_w_gate, expert_w_up, expert_w_down, ln2_g, ln2_b):
    B, S, D = x.shape
    slopes = jnp.exp2(-8.0 * jnp.arange(1, DIL_HEADS + 1, dtype=jnp.float32) / DIL_HEADS)
    h = x
    for l in range(DEPTH):
        proj = jnp.einsum('bsd,dp->bsp', h, w_in[l])
        g_q, g_k, g_v, g_r, g_a, d_q, d_k, d_v = jnp.split(proj, SPLIT_POINTS, axis=-1)
        log_a = jax.nn.log_sigmoid((g_a @ gla_gate_w2[l] + gla_gate_b[l]).astype(jnp.float32)) / GLA_GATE_TEMP
        o_gla = gla_chunked(to_heads(g_q, GLA_HEADS) * (GLA_DK ** -0.5), to_heads(g_k, GLA_HEADS),
                            to_heads(g_v, GLA_HEADS), to_heads(log_a, GLA_HEADS))
        o_gla = from_heads(rms_norm(o_gla, gla_norm_g[l])) * jax.nn.silu(g_r)
        o_dil = dilated_mixture(to_heads(d_q, DIL_HEADS), to_heads(d_k, DIL_HEADS),
                                to_heads(d_v, DIL_HEADS), slopes)
        o_dil = from_heads(rms_norm(o_dil, dil_norm_g[l]))
        mix = jnp.einsum('bsm,md->bsd', jnp.concatenate([o_gla, o_dil], axis=-1), w_out[l])
        h = layer_norm(DEEPNORM_ALPHA * h + mix, ln1_g[l], ln1_b[l])
        ffn = hier_moe(h.reshape(B * S, D), router_coarse_w[l], router_coarse_b[l], router_fine_w[l],
                       router_fine_b[l], expert_w_gate[l], expert_w_up[l], expert_w_down[l])
        h = layer_norm(DEEPNORM_ALPHA * h + ffn.reshape(B, S, D), ln2_g[l], ln2_b[l])
    return h
```

```python
import numpy as np
from contextlib import ExitStack
import concourse.bass as bass
import concourse.mybir as mybir
from concourse.bass_utils import run_bass_kernel_spmd

F32, BF16, I32 = mybir.dt.float32, mybir.dt.bfloat16, mybir.dt.int32
AF = mybir.ActivationFunctionType
ALU = mybir.AluOpType
AX = mybir.AxisListType

NCORES = 8
TOWN = 2048
TALL = 4096
DIL_CFG = ((128, 1), (512, 4), (2048, 16))
EPS = 1e-5
ALPHA = 2.0 ** 0.25
NEXP = 32
CAP = 256
NROWS = NEXP * CAP


class Prog:
    COMPUTE = ("pe", "act", "dve", "pool")

    def __init__(self, nc, es, n_dma_sems=32):
        self.nc = nc
        self.h = {"pe": nc.tensor, "act": nc.scalar, "dve": nc.vector,
                  "pool": nc.gpsimd, "sp": nc.sync}
        self.sem = {e: es.enter_context(nc.semaphore("s_" + e)) for e in self.COMPUTE}
        self.dsem = [es.enter_context(nc.semaphore("d%d" % i)) for i in range(n_dma_sems)]
        self.ops = []
        self.res = {}

    def _deps(self, opid, rk, reads, writes):
        deps = set()
        for k in reads:
            r = self.res.get(k)
            if r is not None and r[0] is not None:
                deps.add(r[0])
        for k in writes:
            r = self.res.get(k)
            if r is not None:
                if r[0] is not None:
                    deps.add(r[0])
                deps.update(r[1].values())
        for k in reads:
            r = self.res.setdefault(k, [None, {}])
            r[1][rk] = opid
        for k in writes:
            self.res[k] = [opid, {}]
        deps.discard(opid)
        return deps

    def capture(self):
        self._cap = []

    def end_capture(self):
        L, self._cap = self._cap, None
        return L

    def replay_interleaved(self, lists):
        lists = [list(L) for L in lists]
        while any(lists):
            for L in lists:
                if L:
                    kind, eng, name, args, kw, r, w = L.pop(0)
                    (self.c if kind == "c" else self.d)(eng, name, *args, r=r, w=w, **kw)

    def c(self, eng, name, *args, r=(), w=(), **kw):
        if getattr(self, "_cap", None) is not None:
            self._cap.append(("c", eng, name, args, kw, r, w))
            return None
        if getattr(self, "skip", False):
            return None
        opid = len(self.ops)
        deps = self._deps(opid, eng, r, w)
        self.ops.append(dict(kind="c", eng=eng, fn=(name, args, kw), deps=deps))
        return opid

    def d(self, q, name, *args, r=(), w=(), **kw):
        if getattr(self, "_cap", None) is not None:
            self._cap.append(("d", q, name, args, kw, r, w))
            return None
        if getattr(self, "skip", False):
            return None
        opid = len(self.ops)
        deps = self._deps(opid, ("dma", opid), r, w)
        self.ops.append(dict(kind="d", eng=q, fn=(name, args, kw), deps=deps))
        return opid

    def flush(self):
        if not hasattr(self, "cnt"):
            self.cnt = {e: 0 for e in self.COMPUTE}
            self.rank = {}
            self.waited = {e: {} for e in self.h}
            self.dcnt = [0] * len(self.dsem)
            self.rr = 0
            self.rr_sw = 0
            self.nwait = 0
            self.done = 0
        ops = self.ops
        lo = self.done
        needed = set()
        last = {}
        for i in range(lo, len(ops)):
            op = ops[i]
            if op["kind"] == "c":
                last[op["eng"]] = i
            for d in op["deps"]:
                if d < lo:
                    continue
                dop = ops[d]
                if dop["kind"] == "c":
                    if dop["eng"] == "pe" and op["kind"] == "c" and op["eng"] == "pe":
                        continue
                    needed.add(d)
        needed.update(last.values())
        cnt, rank, waited, dcnt = self.cnt, self.rank, self.waited, self.dcnt
        for i in range(lo, len(ops)):
            op = ops[i]
            q = op["eng"]
            need = {}
            for d in op["deps"]:
                if d < lo:
                    continue
                dop = ops[d]
                if dop["kind"] == "c":
                    if dop["eng"] == "pe" and op["kind"] == "c" and q == "pe":
                        continue
                    sk, val = dop["eng"], rank[d]
                else:
                    sk, val = dop["dsem"], dop["dval"]
                if waited[q].get(sk, 0) < val:
                    need[sk] = max(need.get(sk, 0), val)
            if op["kind"] == "d":
                half = len(self.dsem) // 2
                if q == "pool":
                    s = self.rr_sw
                    self.rr_sw = (self.rr_sw + 1) % half
                else:
                    s = half + self.rr
                    self.rr = (self.rr + 1) % half
                sk = ("d", s)
                if dcnt[s] > 0 and waited[q].get(sk, 0) < dcnt[s]:
                    need[sk] = max(need.get(sk, 0), dcnt[s])
            for sk, val in need.items():
                sh = self.sem[sk] if isinstance(sk, str) else self.dsem[sk[1]]
                self.h[q].wait_ge(sh, val)
                waited[q][sk] = val
                self.nwait += 1
            name, args, kw = op["fn"]
            ins = getattr(self.h[q], name)(*args, **kw)
            if op["kind"] == "c":
                if i in needed:
                    cnt[q] += 1
                    rank[i] = cnt[q]
                    ins.then_inc(self.sem[q], 1)
            else:
                dcnt[s] += 16
                ins.then_inc(self.dsem[s], 16)
                op["dsem"] = ("d", s)
                op["dval"] = dcnt[s]
        self.done = len(ops)
        for q in self.h:
            for e in self.COMPUTE:
                if cnt[e] > waited[q].get(e, 0):
                    self.h[q].wait_ge(self.sem[e], cnt[e])
                    waited[q][e] = cnt[e]
            for s_, v in enumerate(dcnt):
                if v > waited[q].get(("d", s_), 0):
                    self.h[q].wait_ge(self.dsem[s_], v)
                    waited[q][("d", s_)] = v
        self.res = {}
        self.stats = dict(n_ops=len(ops), n_wait=self.nwait, cnt=dict(cnt), n_dma=sum(dcnt) // 16)
        return self.stats


class RR:
    def __init__(self, items):
        self.items = list(items)
        self.i = 0

    def next(self):
        it = self.items[self.i]
        self.i = (self.i + 1) % len(self.items)
        return it


def build_program(dbg=()):
    nc = bass.Bass("TRN2", target_bir_lowering=False)
    es = ExitStack()
    P = Prog(nc, es)

    def din(name, shape, dt=F32):
        return nc.dram_tensor(name, list(shape), dt, kind="ExternalInput").ap()

    def dout(name, shape, dt=F32):
        return nc.dram_tensor(name, list(shape), dt, kind="ExternalOutput").ap()

    uid = [0]

    def sb(name, shape, dt, st=None):
        uid[0] += 1
        return (st or es).enter_context(nc.sbuf_tensor("sb%d_%s" % (uid[0], name), list(shape), dt))

    xT_d = din("xT", [1024, TALL])
    w_inA_d = din("w_inA", [128, 8, 1536])
    masks_d = din("masks", [128, 24, 256])
    ident_d = din("ident", [128, 128])
    wn_d = din("wn", [65, 64])
    gdil_d = din("gdil", [64, 1])
    pv_d = din("pv", [128, 1])
    w_inB_d = din("w_inB", [128, 8, 1552])
    w2aug_d = din("w2aug", [17, 256])
    gnorm_d = din("gnorm", [128, 128])
    cmat_d = din("cmat", [128, 4, 128])
    w_og_d = din("w_og", [128, 4, 1024])
    w_od_d = din("w_od", [64, 8, 1024])
    rw_d = din("rw", [128, 8, 36])
    rbias_d = din("rbias", [128, 36])
    ln1g_d = din("ln1g", [128, 1024])
    ln1b_d = din("ln1b", [128, 1024])
    ln2g_d = din("ln2g", [128, 1024])
    ln2b_d = din("ln2b", [128, 1024])
    xtok_d = din("xtok", [TOWN, 1024])
    wg_d = din("wg", [NEXP, 128, 8, 512])
    wu_d = din("wu", [NEXP, 128, 8, 512])
    wd_d = din("wd", [NEXP, 128, 4, 1024])
    out_d = dout("out", [TOWN, 1024])
    rconst_d = din("rconst", [128, 3, 128])
    xbuf = nc.dram_tensor("xbuf", [NROWS, 1024], BF16, kind="Internal").ap()
    ybuf = nc.dram_tensor("ybuf", [NROWS, 1024], BF16, kind="Internal").ap()
    h1buf = nc.dram_tensor("h1buf", [TOWN, 1024], F32, kind="Internal").ap()
    wgb = nc.dram_tensor("wgb", [NEXP, 128, 4096], BF16, kind="Internal").ap()
    wub = nc.dram_tensor("wub", [NEXP, 128, 4096], BF16, kind="Internal").ap()
    wdb = nc.dram_tensor("wdb", [NEXP, 128, 4096], BF16, kind="Internal").ap()
    pc_i = [0]

    def precast(n=1):
        for _ in range(n):
            k = pc_i[0]
            if k >= 3 * NEXP:
                return
            pc_i[0] += 1
            e_, m_ = k // 3, k % 3
            src = (wg_d, wu_d, wd_d)[m_][e_].rearrange("p c f -> p (c f)")
            dst = (wgb, wub, wdb)[m_][e_]
            P.d("pool", "dma_start", out=dst, in_=src, w=[("wb", k)])
    bc_reg = nc.gpsimd.to_reg(NROWS - 1)
    if "odil" in dbg:
        odil_o = dout("odil_dbg", [64, 8, TOWN])
    if "ogla" in dbg:
        ogla_o = dout("ogla_dbg", [128, 4, TOWN])

    ident = sb("ident", [128, 128], BF16)
    wn = sb("wn", [65, 64], BF16)
    gdil = sb("gdil", [64, 1], F32)
    pvt = sb("pvt", [128, 1], F32)
    o_dilT = sb("o_dilT", [64, 8, TOWN], BF16)
    o_glaT = sb("o_glaT", [128, 4, TOWN], BF16)

    ps = [es.enter_context(nc.psum_tensor("ps%d" % i, [128, 512], F32)) for i in range(7)]
    psb = es.enter_context(nc.psum_tensor("psb", [128, 1024], BF16))
    ps_rr = RR([(ps[i], ("ps", i)) for i in range(7)])

    P.d("pool", "dma_start", out=ident[:], in_=ident_d, w=["ident"])
    P.d("pool", "dma_start", out=wn[:], in_=wn_d, w=["wn"])
    P.d("sp", "dma_start", out=gdil[:], in_=gdil_d, w=["gdil"])
    P.d("sp", "dma_start", out=pvt[:], in_=pv_d, w=["pvt"])
    P.flush()

    P.skip = "skipA" in dbg
    ph = ExitStack()
    xs = [sb("xs%d" % i, [128, 8, 512], BF16, ph) for i in range(2)]
    w_inB = sb("w_inB", [128, 8, 1552], BF16, ph)
    w2aug = sb("w2aug", [17, 256], BF16, ph)
    gnorm = sb("gnorm", [128, 128], F32, ph)
    cmat = sb("cmat", [128, 4, 128], F32, ph)
    causal_bf = sb("causal_bf", [128, 128], BF16, ph)
    epsc = sb("epsc", [128, 1], F32, ph)
    gaTs = [sb("gaT%d" % i, [32, 512], BF16, ph) for i in range(2)]
    qkTs = [sb("qkT%d" % i, [128, 4, 512], F32, ph) for i in range(2)]
    Sst = sb("Sst", [128, 2, 128], F32, ph)
    Sbf = [sb("Sbf%d" % i, [128, 2, 128], BF16, ph) for i in range(2)]
    NB = 4
    e1 = [sb("e1_%d" % i, [128, 256], F32, ph) for i in range(NB)]
    lap = [sb("lap%d" % i, [128, 256], F32, ph) for i in range(NB)]
    lhl = [sb("lhl%d" % i, [128, 2, 256], BF16, ph) for i in range(NB)]
    cmb = sb("cmb", [128, 4, 128], BF16, ph)
    dec = [sb("dec%d" % i, [128, 2], F32, ph) for i in range(NB)]
    ek = [sb("ek%d" % i, [128, 256], F32, ph) for i in range(NB)]
    khat = [sb("khat%d" % i, [128, 256], BF16, ph) for i in range(NB)]
    vbf = [sb("vbf%d" % i, [128, 512], BF16, ph) for i in range(NB)]
    gs = [sb("gs%d" % i, [128, 512], F32, ph) for i in range(NB)]
    ebT = [sb("ebT%d" % i, [128, 2, 128], F32, ph) for i in range(NB)]
    enbT = [sb("enbT%d" % i, [128, 2, 128], F32, ph) for i in range(NB)]
    qtl = [sb("qtl%d" % i, [128, 2, 2, 128], BF16, ph) for i in range(NB)]
    ktl = [sb("ktl%d" % i, [128, 2, 128], BF16, ph) for i in range(NB)]
    Am = [sb("Am%d" % i, [128, 4, 128], BF16, ph) for i in range(NB)]
    sqo = [sb("sqo%d" % i, [128, 512], F32, ph) for i in range(NB)]
    ssr = [sb("ssr%d" % i, [128, 4], F32, ph) for i in range(NB)]
    onrm = [sb("onrm%d" % i, [128, 512], F32, ph) for i in range(NB)]
    ogt = [sb("ogt%d" % i, [128, 512], BF16, ph) for i in range(NB)]
    zt = sb("zt", [128, 8192], BF16, ph)
    P.c("pool", "memset", zt[:], 0.0, w=["zt"])
    xz = xbuf.rearrange("(c p j) d -> c p (j d)", p=128, j=8)
    for c_ in range(NROWS // 1024):
        P.d("sp", "dma_start", out=xz[c_], in_=zt[:], r=["zt"], w=[("xbufz", c_)])
    P.d("pool", "dma_start", out=w_inB[:], in_=w_inB_d, w=["w_inB"])
    P.d("pool", "dma_start", out=w2aug[:], in_=w2aug_d, w=["w2aug"])
    P.d("sp", "dma_start", out=gnorm[:], in_=gnorm_d, w=["gnorm"])
    P.d("sp", "dma_start", out=cmat[:], in_=cmat_d, w=["cmat"])
    P.c("dve", "tensor_copy", out=causal_bf[:], in_=cmat[:, 2, :], r=["cmat"], w=["causal_bf"])
    P.c("dve", "tensor_copy", out=cmb[:], in_=cmat[:], r=["cmat"], w=["cmb"])
    P.c("dve", "memset", epsc[:], EPS, w=["epsc"])
    for i in range(2):
        P.c("dve", "memset", gaTs[i][:], 1.0, w=[("gaT", i)])
    P.c("dve", "memset", Sst[:], 0.0, w=["Sst"])
    for i in range(2):
        P.c("dve", "memset", Sbf[i][:], 0.0, w=[("Sbf", i)])
    for i in range(NB):
        P.c("pool", "memset", qtl[i][:], 0.0, w=[("qtl", i)])
    xT_v = xT_d.rearrange("(c p) t -> p c t", p=128)
    TRI, RTRI, CAUS, ONEG = 0, 1, 2, 3
    def group_ops(g):
        own = g >= 4
        xb = xs[g % 2]
        xk = ("xs", g % 2)
        gat = gaTs[g % 2]
        gak = ("gaT", g % 2)
        qk_ = qkTs[g % 2]
        P.d("pool", "dma_start", out=xb[:], in_=xT_v[:, :, g * 512:(g + 1) * 512], w=[xk])
        pst, psk = ps_rr.next()
        for dc in range(8):
            P.c("pe", "matmul", pst[0:16, 0:512], w_inB[:, dc, 1536:1552], xb[:, dc, :],
                start=(dc == 0), stop=(dc == 7), r=["w_inB", xk], w=[psk])
        P.c("act", "copy", out=gat[0:16, :], in_=pst[0:16, 0:512], r=[psk], w=[gak])
        if own:
            for ct in range(4):
                pst, psk = ps_rr.next()
                for dc in range(8):
                    P.c("pe", "matmul", pst[:, 0:512], w_inB[:, dc, ct * 128:(ct + 1) * 128], xb[:, dc, :],
                        start=(dc == 0), stop=(dc == 7), r=["w_inB", xk], w=[psk])
                P.c("act", "copy", out=qk_[:, ct, :], in_=pst[:, 0:512], r=[psk], w=[("qkT", g % 2, ct)])

    def Pst(c):
        g, j = c // 4, c % 4
        own = g >= 4
        xb = xs[g % 2]
        xk = ("xs", g % 2)
        gat = gaTs[g % 2]
        gak = ("gaT", g % 2)
        qk_ = qkTs[g % 2]
        b = c % NB
        tok = slice(j * 128, (j + 1) * 128)
        zps, zk = cur_rr[0].next()
        P.c("pe", "matmul", zps[:, 0:256], gat[0:17, tok], w2aug[0:17, :], start=True, stop=True,
            r=[gak, "w2aug"], w=[zk])
        P.c("act", "activation", out=e1[b][:], in_=zps[:, 0:256], func=AF.Exp, scale=-1.0,
            r=[zk], w=[("e1", b)])
        P.c("act", "activation", out=lap[b][:], in_=e1[b][:], func=AF.Ln, bias=1.0,
            r=[("e1", b)], w=[("lap", b)])
        P.c("dve", "tensor_copy", out=lhl[b][:, 0, :], in_=lap[b][:], r=[("lap", b)], w=[("lhl", b)])
        P.c("dve", "tensor_tensor", out=lhl[b][:, 1, :], in0=lap[b][:], in1=lhl[b][:, 0, :], op=ALU.subtract,
            r=[("lap", b), ("lhl", b)], w=[("lhl", b)])
        dps, dk_ = cur_rr[0].next()
        for T in range(2):
            for hl_ in range(2):
                P.c("pe", "matmul", dps[:, T:T + 1], lhl[b][:, hl_, T * 128:(T + 1) * 128], cmb[:, ONEG, 0:1],
                    start=(hl_ == 0), stop=(hl_ == 1), r=[("lhl", b), "cmb"], w=[dk_])
        for hl_ in range(2):
            P.c("pe", "matmul", dps[:, 256:512], cmb[:, RTRI, :], lhl[b][:, hl_, :],
                start=(hl_ == 0), stop=(hl_ == 1), r=[("lhl", b), "cmb"], w=[dk_])
        P.c("act", "activation", out=dec[b][:], in_=dps[:, 0:2], func=AF.Exp, r=[dk_], w=[("dec", b)])
        P.c("act", "activation", out=ek[b][:], in_=dps[:, 256:512], func=AF.Exp, r=[dk_], w=[("ek", b)])
        kps, kk = cur_rr[0].next()
        for dc in range(8):
            P.c("pe", "matmul", kps[:, 0:256], xb[:, dc, tok], w_inB[:, dc, 256:512],
                start=(dc == 0), stop=(dc == 7), r=["w_inB", xk], w=[kk])
        P.c("dve", "tensor_tensor", out=khat[b][:], in0=kps[:, 0:256], in1=ek[b][:], op=ALU.mult,
            r=[kk, ("ek", b)], w=[("khat", b)])
        vps, vk = cur_rr[0].next()
        for dc in range(8):
            P.c("pe", "matmul", vps[:, 0:512], xb[:, dc, tok], w_inB[:, dc, 512:1024],
                start=(dc == 0), stop=(dc == 7), r=["w_inB", xk], w=[vk])
        P.c("act", "copy", out=vbf[b][:], in_=vps[:, 0:512], r=[vk], w=[("vbf", b)])
        if not own:
            return
        gps, gk = cur_rr[0].next()
        for dc in range(8):
            P.c("pe", "matmul", gps[:, 0:512], xb[:, dc, tok], w_inB[:, dc, 1024:1536],
                start=(dc == 0), stop=(dc == 7), r=["w_inB", xk], w=[gk])
        P.c("act", "activation", out=gs[b][:], in_=gps[:, 0:512], func=AF.Silu, r=[gk], w=[("gs", b)])
        P.c("pool", "tensor_tensor", out=gs[b][:].rearrange("p (h e) -> p h e", h=4),
            in0=gs[b][:].rearrange("p (h e) -> p h e", h=4),
            in1=gnorm[:].unsqueeze(1).to_broadcast([128, 4, 128]), op=ALU.mult,
            r=[("gs", b), "gnorm"], w=[("gs", b)])
        bps, bk = cur_rr[0].next()
        for T in range(2):
            for hl_ in range(2):
                P.c("pe", "matmul", bps[:, T * 128:(T + 1) * 128], lhl[b][:, hl_, T * 128:(T + 1) * 128],
                    cmb[:, TRI, :], start=(hl_ == 0), stop=(hl_ == 1), r=[("lhl", b), "cmb"], w=[bk])
        P.c("act", "activation", out=ebT[b][:].rearrange("p t k -> p (t k)"), in_=bps[:, 0:256],
            func=AF.Exp, r=[bk], w=[("ebT", b)])
        P.c("act", "activation", out=enbT[b][:].rearrange("p t k -> p (t k)"), in_=bps[:, 0:256],
            func=AF.Exp, scale=-1.0, r=[bk], w=[("enbT", b)])
        qkk = [("qkT", g % 2, ct) for ct in range(4)]
        for hh in range(2):
            pr = slice(hh * 64, (hh + 1) * 64)
            P.c("dve", "scalar_tensor_tensor", out=qtl[b][pr, hh, :, :], in0=qk_[pr, 0:2, tok], scalar=0.125,
                in1=ebT[b][pr, :, :], op0=ALU.mult, op1=ALU.mult, r=qkk[0:2] + [("ebT", b)], w=[("qtl", b)])
        P.c("dve", "tensor_tensor", out=ktl[b][:], in0=qk_[:, 2:4, tok], in1=enbT[b][:], op=ALU.mult,
            r=qkk[2:4] + [("enbT", b)], w=[("ktl", b)])
        aps, ak = cur_rr[0].next()
        for h in range(4):
            T, hh = h // 2, h % 2
            P.c("pe", "matmul", aps[:, h * 128:(h + 1) * 128], ktl[b][:, T, :], qtl[b][:, hh, T, :],
                start=True, stop=True, r=[("ktl", b), ("qtl", b)], w=[ak])
        P.c("dve", "tensor_tensor", out=Am[b][:], in0=aps[:, 0:512].rearrange("p (h i) -> p h i", h=4),
            in1=causal_bf[:].unsqueeze(1).to_broadcast([128, 4, 128]), op=ALU.mult,
            r=[ak, "causal_bf"], w=[("Am", b)])

    def Gst(c):
        g, j = c // 4, c % 4
        own = g >= 4
        b = c % NB
        sb_old = c % 2
        sb_new = (c + 1) % 2
        if own:
            t0 = (g - 4) * 512 + j * 128
            ops_, ok_ = cur_rr[0].next()
            for h in range(4):
                T, hh = h // 2, h % 2
                P.c("pe", "matmul", ops_[:, h * 128:(h + 1) * 128], qtl[b][:, hh, T, :],
                    Sbf[sb_old][:, T, :], start=True, stop=False,
                    r=[("qtl", b), ("Sbf", sb_old)], w=[ok_])
                P.c("pe", "matmul", ops_[:, h * 128:(h + 1) * 128], Am[b][:, h, :],
                    vbf[b][:, h * 128:(h + 1) * 128], start=False, stop=True,
                    r=[("Am", b), ("vbf", b)], w=[ok_])
            P.c("act", "activation", out=sqo[b][:], in_=ops_[:, 0:512], func=AF.Square,
                r=[ok_], w=[("sqo", b)])
            P.c("dve", "tensor_reduce", out=ssr[b][:], in_=sqo[b][:].rearrange("p (h e) -> p h e", h=4),
                axis=AX.X, op=ALU.add, r=[("sqo", b)], w=[("ssr", b)])
            P.c("act", "activation", out=ssr[b][:], in_=ssr[b][:], func=AF.Ln, scale=1.0 / 128,
                bias=epsc[:, 0:1], r=[("ssr", b), "epsc"], w=[("ssr", b)])
            P.c("act", "activation", out=ssr[b][:], in_=ssr[b][:], func=AF.Exp, scale=-0.5,
                r=[("ssr", b)], w=[("ssr", b)])
            P.c("dve", "tensor_tensor", out=onrm[b][:].rearrange("p (h e) -> p h e", h=4),
                in0=ops_[:, 0:512].rearrange("p (h e) -> p h e", h=4),
                in1=ssr[b][:].unsqueeze(2).to_broadcast([128, 4, 128]), op=ALU.mult,
                r=[ok_, ("ssr", b)], w=[("onrm", b)])
            P.c("pool", "tensor_tensor", out=ogt[b][:], in0=onrm[b][:], in1=gs[b][:], op=ALU.mult,
                r=[("onrm", b), ("gs", b)], w=[("ogt", b)])
            tpg, tpgk = cur_rr[0].next()
            for h in range(4):
                P.c("pe", "matmul", tpg[:, h * 128:(h + 1) * 128], ogt[b][:, h * 128:(h + 1) * 128], ident[:],
                    start=True, stop=True, r=["ident", ("ogt", b)], w=[tpgk])
            P.c("act", "copy", out=o_glaT[:, :, t0:t0 + 128],
                in_=tpg[:, 0:512].rearrange("p (h t) -> p h t", h=4), r=[tpgk], w=[("ogla", t0 // 128)])
        for T in range(2):
            sps_, sk_ = cur_rr[0].next()
            P.c("pe", "matmul", sps_[:, 0:256], khat[b][:, T * 128:(T + 1) * 128],
                vbf[b][:, T * 256:(T + 1) * 256], start=True, stop=True,
                r=[("khat", b), ("vbf", b)], w=[sk_])
            for hh in range(2):
                pr = slice(hh * 64, (hh + 1) * 64)
                P.c("dve", "scalar_tensor_tensor", out=Sst[pr, T, :], in0=Sst[pr, T, :],
                    scalar=dec[b][pr, T:T + 1], in1=sps_[pr, hh * 128:(hh + 1) * 128],
                    op0=ALU.mult, op1=ALU.add, r=["Sst", ("dec", b), sk_], w=["Sst"])
        P.c("act", "copy", out=Sbf[sb_new][:], in_=Sst[:], r=["Sst"], w=[("Sbf", sb_new)])

    rrA = RR([(ps[i], ("ps", i)) for i in (0, 1, 2)])
    rrB = RR([(ps[i], ("ps", i)) for i in (3, 4, 5)])
    rrG = RR([(ps[i], ("ps", i)) for i in (6,)])
    cur_rr = [ps_rr]
    NCH = TALL // 128
    for pi in range(NCH // 2):
        c0_ = 2 * pi
        if c0_ % 4 == 0:
            group_ops(c0_ // 4)
        streams = []
        for cc, rr_ in ((c0_, rrA), (c0_ + 1, rrB)):
            cur_rr[0] = rr_
            P.capture()
            Pst(cc)
            streams.append(P.end_capture())
        if pi > 0:
            cur_rr[0] = rrG
            P.capture()
            Gst(c0_ - 2)
            Gst(c0_ - 1)
            streams.append(P.end_capture())
        P.replay_interleaved(streams)
        precast(2)
    cur_rr[0] = ps_rr
    Gst(NCH - 2)
    Gst(NCH - 1)

    if "ogla" in dbg:
        P.d("pool", "dma_start", out=ogla_o, in_=o_glaT[:], r=[("ogla", i) for i in range(16)], w=["ogla_o"])

    P.flush()
    ph.close()

    P.skip = "skipB" in dbg
    ph = ExitStack()
    KT = sb("KT", [128, 4, TALL], BF16, ph)
    VT = sb("VT", [128, 4, TALL], BF16, ph)
    QT = sb("QT", [128, 4, TOWN], BF16, ph)
    ph2 = ExitStack()
    xs = [sb("xs%d" % i, [128, 8, 512], BF16, ph2) for i in range(2)]
    w_inA = sb("w_inA", [128, 8, 1536], BF16, ph2)
    P.d("pool", "dma_start", out=w_inA[:], in_=w_inA_d, w=["w_inA"])
    evq = RR(["act", "dve"])
    for g in range(TALL // 512):
        xb = xs[g % 2]
        P.d("pool", "dma_start", out=xb[:], in_=xT_v[:, :, g * 512:(g + 1) * 512], w=[("xs", g % 2)])
        precast(2)
        tiles = [("K", ct) for ct in range(4)] + [("V", ct) for ct in range(4)]
        if g >= 4:
            tiles += [("Q", ct) for ct in range(4)]
        for (which, ct) in tiles:
            col0 = {"Q": 0, "K": 512, "V": 1024}[which] + ct * 128
            pst, psk = ps_rr.next()
            for dc in range(8):
                P.c("pe", "matmul", pst[:, 0:512], w_inA[:, dc, col0:col0 + 128], xb[:, dc, :],
                    start=(dc == 0), stop=(dc == 7), r=["w_inA", ("xs", g % 2)], w=[psk])
            if which == "Q":
                dst = QT[:, ct, (g - 4) * 512:(g - 3) * 512]
            elif which == "K":
                dst = KT[:, ct, g * 512:(g + 1) * 512]
            else:
                dst = VT[:, ct, g * 512:(g + 1) * 512]
            e = evq.next()
            if e == "act":
                P.c("act", "copy", out=dst, in_=pst[:, 0:512], r=[psk], w=[(which, ct, g)])
            else:
                P.c("dve", "tensor_copy", out=dst, in_=pst[:, 0:512], r=[psk], w=[(which, ct, g)])

    def kv_keys(which, ct, tok0, r):
        g0 = tok0 // 512
        g1 = (tok0 + 127 * r) // 512
        return [(which, ct, g) for g in range(g0, g1 + 1)]

    P.flush()
    ph2.close()
    ph2 = ExitStack()
    masks = sb("masks", [128, 24, 256], BF16, ph2)
    acc = sb("acc", [65, 2, TOWN], F32, ph2)
    sq = sb("sq", [65, TOWN], BF16, ph2)
    rstd = [sb("rstd%d" % i, [64, 512], F32, ph2) for i in range(2)]
    Et = [sb("Et%d" % i, [128, 512], BF16, ph2) for i in range(7)]
    Pt = [sb("Pt%d" % i, [128, 2, 256], BF16, ph2) for i in range(7)]
    vs_own = [sb("vso%d" % i, [128, 2, 65], BF16, ph2) for i in range(18)]
    vs_pre = [sb("vsp%d" % i, [128, 2, 65], BF16, ph2) for i in range(14)]
    P.d("pool", "dma_start", out=masks[:], in_=masks_d, w=["masks"])
    for i, t in enumerate(vs_own):
        P.c("dve", "memset", t[:], 1.0, w=[("vso", i)])
    for i, t in enumerate(vs_pre):
        P.c("dve", "memset", t[:], 0.0, w=[("vsp", i)])
        for hl in range(2):
            P.c("dve", "tensor_copy", out=t[:, hl, 64:65], in_=pvt[:, 0:1], r=["pvt"], w=[("vsp", i)])
    et_rr = RR([(Et[i], ("Et", i)) for i in range(7)])
    pt_rr = RR([(Pt[i], ("Pt", i)) for i in range(7)])
    own_rr = RR(list(enumerate(vs_own)))
    pre_rr = RR(list(enumerate(vs_pre)))
    vt_slot = [-1]
    vt_bank = [None]
    for hp in range(4):
        items = []
        for ci, (window, r) in enumerate(DIL_CFG):
            nblk = TALL // r // 128
            own_n0 = nblk // 2
            if r == 1:
                quads = [[(0, n) for n in range(n0, n0 + 4)] for n0 in range(own_n0, nblk, 4)]
            elif r == 4:
                quads = [[(c, n) for n in range(own_n0, nblk)] for c in range(4)]
            else:
                quads = [[(c, 1) for c in range(c0, c0 + 4)] for c0 in range(0, 16, 4)]
            vcache = {}

            def get_vs(c, n, hp=hp, r=r, own_n0=own_n0, vcache=vcache, own_rr=own_rr, pre_rr=pre_rr):
                if (c, n) in vcache:
                    return vcache[(c, n)]
                is_pre = n < own_n0
                idx, t = (pre_rr if is_pre else own_rr).next()
                key = ("vsp" if is_pre else "vso", idx)
                tok0 = c + r * 128 * n
                vt_slot[0] = (vt_slot[0] + 1) % 4
                if vt_slot[0] == 0 or vt_bank[0] is None:
                    vt_bank[0] = ps_rr.next()
                vtp, vtk = vt_bank[0]
                sl = slice(vt_slot[0] * 128, (vt_slot[0] + 1) * 128)
                P.c("pe", "matmul", vtp[:, sl], VT[:, hp, tok0:tok0 + 127 * r + 1:r], ident[:],
                    start=True, stop=True, r=["ident"] + kv_keys("V", hp, tok0, r), w=[vtk])
                P.c("act", "copy", out=t[:, :, 0:64], in_=vtp[:, sl].rearrange("p (h d) -> p h d", h=2),
                    r=[vtk], w=[key])
                vcache[(c, n)] = (t, key)
                return t, key

            for quad in quads:
                for hl in range(2):
                    items.append((ci, r, own_n0, quad, hl, get_vs))
        st1 = {}

        def stage1(n):
            ci, r, own_n0, quad, hl, get_vs = items[n]
            vsl = [(get_vs(c, nn - 1), get_vs(c, nn)) for (c, nn) in quad]
            h = hp * 2 + hl
            pb = hl * 64
            pts = []
            for pair in range(2):
                sps, spk = ps_rr.next()
                for u in range(2):
                    c, nn = quad[pair * 2 + u]
                    tq = c + r * 128 * nn
                    tk = c + r * 128 * (nn - 1)
                    qv = QT[pb:pb + 64, hp, tq - TOWN:tq - TOWN + 127 * r + 1:r]
                    kprev = KT[pb:pb + 64, hp, tk:tk + 127 * r + 1:r]
                    kcur = KT[pb:pb + 64, hp, tq:tq + 127 * r + 1:r]
                    P.c("pe", "matmul", sps[:, u * 256:u * 256 + 128], kprev, qv, start=True, stop=True,
                        r=kv_keys("K", hp, tk, r) + kv_keys("Q", hp, tq, r), w=[spk])
                    P.c("pe", "matmul", sps[:, u * 256 + 128:u * 256 + 256], kcur, qv, start=True, stop=True,
                        r=kv_keys("K", hp, tq, r) + kv_keys("Q", hp, tq, r), w=[spk])
                et, etk = et_rr.next()
                P.c("act", "activation", out=et[:], in_=sps[:, 0:512], func=AF.Exp, scale=0.125,
                    r=[spk], w=[etk])
                pt, ptk = pt_rr.next()
                mi = ci * 8 + h
                P.c("dve", "tensor_tensor", out=pt[:], in0=et[:].rearrange("p (u k) -> p u k", u=2),
                    in1=masks[:, mi:mi + 1, :].to_broadcast([128, 2, 256]), op=ALU.mult,
                    r=[etk, "masks"], w=[ptk])
                pts.append((pt, ptk))
            st1[n] = (vsl, pts)

        def stage2(n):
            ci, r, own_n0, quad, hl, get_vs = items[n]
            vsl, pts = st1.pop(n)
            ups, upk = ps_rr.next()
            for pair in range(2):
                pt, ptk = pts[pair]
                for u in range(2):
                    qi = pair * 2 + u
                    (vp, vpk), (vc, vck) = vsl[qi]
                    P.c("pe", "matmul", ups[0:65, qi * 128:(qi + 1) * 128], vp[:, hl, :], pt[:, u, 0:128],
                        start=True, stop=False, r=[vpk, ptk], w=[upk])
                    P.c("pe", "matmul", ups[0:65, qi * 128:(qi + 1) * 128], vc[:, hl, :], pt[:, u, 128:256],
                        start=False, stop=True, r=[vck, ptk], w=[upk])
            if r == 1:
                n0 = quad[0][1] - own_n0
                dst = acc[0:65, hl, n0 * 128:n0 * 128 + 512]
                P.c("act", "copy", out=dst, in_=ups[0:65, 0:512], r=[upk], w=[("acc", hl)])
            else:
                if r == 4:
                    c = quad[0][0]
                    dst = acc[0:65, hl, c:c + 4 * 511 + 1:4]
                    src = ups[0:65, 0:512]
                else:
                    c0 = quad[0][0]
                    dst = acc[0:65, hl, :].rearrange("p (i c) -> p c i", c=16)[:, c0:c0 + 4, :]
                    src = ups[0:65, 0:512].rearrange("p (c i) -> p c i", c=4)
                P.c("dve", "tensor_tensor", out=dst, in0=dst, in1=src, op=ALU.add,
                    r=[upk, ("acc", hl)], w=[("acc", hl)])

        stage1(0)
        stage1(1)
        for n in range(len(items)):
            if n + 2 < len(items):
                stage1(n + 2)
            stage2(n)
            if n % 3 == 0:
                precast(1)
        for hl in range(2):
            h = hp * 2 + hl
            P.c("act", "activation", out=sq[:], in_=acc[0:65, hl, :], func=AF.Square,
                r=[("acc", hl)], w=["sq"])
            for ch in range(TOWN // 512):
                nps, npk = ps_rr.next()
                P.c("pe", "matmul", nps[0:64, 0:512], wn[:, :], sq[:, ch * 512:(ch + 1) * 512],
                    start=True, stop=True, r=["wn", "sq"], w=[npk])
                rs = rstd[ch % 2]
                P.c("act", "activation", out=rs[:], in_=nps[0:64, 0:512], func=AF.Ln,
                    r=[npk], w=[("rstd", ch % 2)])
                P.c("act", "activation", out=rs[:], in_=rs[:], func=AF.Exp, scale=-0.5,
                    r=[("rstd", ch % 2)], w=[("rstd", ch % 2)])
                P.c("dve", "scalar_tensor_tensor", out=o_dilT[:, h, ch * 512:(ch + 1) * 512],
                    in0=acc[0:64, hl, ch * 512:(ch + 1) * 512], scalar=gdil[:, 0:1], in1=rs[:],
                    op0=ALU.mult, op1=ALU.mult,
                    r=[("acc", hl), ("rstd", ch % 2), "gdil"], w=[("odil", h)])

    if "odil" in dbg:
        P.d("pool", "dma_start", out=odil_o, in_=o_dilT[:], r=[("odil", h) for h in range(8)], w=["odil_o"])

    P.flush()
    ph2.close()
    ph.close()

    P.skip = False
    phC = ExitStack()
    gk_all = sb("gk_all", [128, 16, 2], F32, phC)
    dest_i = sb("dest_i", [128, 32], I32, phC)
    ph = ExitStack()
    h1Ts = [sb("h1T%d" % i, [128, 8, 128], BF16, ph) for i in range(2)]
    rconst = sb("rconst", [128, 3, 128], F32, ph)
    rcb = sb("rcb", [128, 2, 128], BF16, ph)
    ohsum = sb("ohsum", [128, 32], F32, ph)
    ohsb = sb("ohsb", [128, 32], BF16, ph)
    ohb = [sb("ohb%d" % i, [128, 32], BF16, ph) for i in range(2)]
    dest_f = [sb("dest_f%d" % i, [128, 2], F32, ph) for i in range(2)]
    w_og = sb("w_og", [128, 4, 1024], BF16, ph)
    w_od = sb("w_od", [64, 8, 1024], BF16, ph)
    rw = sb("rw", [128, 8, 36], BF16, ph)
    rbias = sb("rbias", [128, 36], F32, ph)
    ln1g = sb("ln1g", [128, 1024], F32, ph)
    ln1b = sb("ln1b", [128, 1024], F32, ph)
    epsc2 = sb("epsc2", [128, 1], F32, ph)
    xt = [sb("xt%d" % i, [128, 1024], F32, ph) for i in range(2)]
    ah = [sb("ah%d" % i, [128, 1024], F32, ph) for i in range(2)]
    hpre = [sb("hpre%d" % i, [128, 1024], F32, ph) for i in range(2)]
    scr = [sb("scr%d" % i, [128, 1024], F32, ph) for i in range(2)]
    h1f = [sb("h1f%d" % i, [128, 1024], F32, ph) for i in range(2)]
    h1b = [sb("h1b%d" % i, [128, 1024], BF16, ph) for i in range(4)]
    st = [sb("st%d" % i, [128, 16], F32, ph) for i in range(2)]
    lg = [sb("lg%d" % i, [128, 36], F32, ph) for i in range(4)]
    rt = [sb("rt%d" % i, [128, 4, 32], F32, ph) for i in range(4)]
    P.d("pool", "dma_start", out=w_og[:], in_=w_og_d, w=["w_og"])
    P.d("pool", "dma_start", out=w_od[:], in_=w_od_d, w=["w_od"])
    P.d("pool", "dma_start", out=rw[:], in_=rw_d, w=["rw"])
    P.d("sp", "dma_start", out=rbias[:], in_=rbias_d, w=["rbias"])
    P.d("sp", "dma_start", out=ln1g[:], in_=ln1g_d, w=["ln1g"])
    P.d("sp", "dma_start", out=ln1b[:], in_=ln1b_d, w=["ln1b"])
    P.c("dve", "memset", epsc2[:], EPS, w=["epsc2"])
    P.d("sp", "dma_start", out=rconst[:], in_=rconst_d, w=["rconst"])
    P.c("dve", "tensor_copy", out=rcb[:], in_=rconst[:, 0:2, :], r=["rconst"], w=["rcb"])
    P.c("dve", "memset", ohsum[:], 0.0, w=["ohsum"])
    P.c("dve", "memset", ohsb[:], 0.0, w=["ohsb"])

    def layer_norm(b, src, srck, dst, dstk, gt, gk, bt, bk, stt, stk, sc, sck):
        P.c("dve", "tensor_reduce", out=stt[:, 0:1], in_=src, axis=AX.X, op=ALU.add, r=[srck], w=[stk])
        P.c("dve", "tensor_scalar", out=stt[:, 1:2], in0=stt[:, 0:1], scalar1=-1.0 / 1024, scalar2=None,
            op0=ALU.mult, r=[stk], w=[stk])
        P.c("act", "activation", out=sc, in_=src, func=AF.Identity, bias=stt[:, 1:2], scale=1.0,
            r=[srck, stk], w=[sck])
        P.c("act", "activation", out=dst, in_=sc, func=AF.Square, r=[sck], w=[dstk])
        P.c("dve", "tensor_reduce", out=stt[:, 2:3], in_=dst, axis=AX.X, op=ALU.add, r=[dstk], w=[stk])
        P.c("act", "activation", out=stt[:, 3:4], in_=stt[:, 2:3], func=AF.Ln, scale=1.0 / 1024,
            bias=epsc2[:, 0:1], r=[stk, "epsc2"], w=[stk])
        P.c("act", "activation", out=stt[:, 4:5], in_=stt[:, 3:4], func=AF.Exp, scale=-0.5, r=[stk], w=[stk])
        P.c("dve", "scalar_tensor_tensor", out=dst, in0=sc, scalar=stt[:, 4:5], in1=gt,
            op0=ALU.mult, op1=ALU.mult, r=[sck, stk, gk], w=[dstk])
        P.c("dve", "tensor_tensor", out=dst, in0=dst, in1=bt, op=ALU.add, r=[dstk, bk], w=[dstk])

    def tileX(i):
        b = i % 2
        b4 = i % 4
        h1T = h1Ts[b]
        tk = slice(i * 128, (i + 1) * 128)
        P.d("sp", "dma_start", out=xt[b][:], in_=xtok_d[tk, :], w=[("xt", b)])
        for half in range(2):
            cs_ = slice(half * 512, (half + 1) * 512)
            mps, mk = cur_rr[0].next()
            for c in range(4):
                P.c("pe", "matmul", mps[:, 0:512], o_glaT[:, c, tk], w_og[:, c, cs_], start=(c == 0), stop=False,
                    r=["w_og", ("ogla", i)], w=[mk])
            for h in range(8):
                P.c("pe", "matmul", mps[:, 0:512], o_dilT[0:64, h, tk], w_od[0:64, h, cs_], start=False,
                    stop=(h == 7), r=["w_od", ("odil", h)], w=[mk])
            P.c("dve", "scalar_tensor_tensor", out=hpre[b][:, cs_], in0=xt[b][:, cs_], scalar=ALPHA,
                in1=mps[:, 0:512], op0=ALU.mult, op1=ALU.add, r=[("xt", b), mk], w=[("hpre", b)])
        layer_norm(b, hpre[b][:], ("hpre", b), h1f[b][:], ("h1f", b), ln1g[:], "ln1g", ln1b[:], "ln1b",
                   st[b], ("st", b), scr[b][:], ("scr", b))
        P.c("act", "mul", out=ah[b][:], in_=h1f[b][:], mul=ALPHA, r=[("h1f", b)], w=[("ah", b)])
        P.d("sp", "dma_start", out=h1buf[tk, :], in_=ah[b][:], r=[("ah", b)], w=[("h1buf", i)])
        P.c("dve", "tensor_copy", out=h1b[b4][:], in_=h1f[b][:], r=[("h1f", b)], w=[("h1b", b4)])
        for hq in range(2):
            tps_, tpk_ = cur_rr[0].next()
            for d4 in range(4):
                dc = hq * 4 + d4
                P.c("pe", "matmul", tps_[:, d4 * 128:(d4 + 1) * 128], h1b[b4][:, dc * 128:(dc + 1) * 128],
                    ident[:], start=True, stop=True, r=["ident", ("h1b", b4)], w=[tpk_])
            P.c("act", "copy", out=h1T[:, hq * 4:hq * 4 + 4, :],
                in_=tps_[:, 0:512].rearrange("p (c t) -> p c t", c=4), r=[tpk_], w=[("h1T", b)])
        lps, lk = cur_rr[0].next()
        for dc in range(8):
            P.c("pe", "matmul", lps[:, 0:36], h1T[:, dc, :], rw[:, dc, :], start=(dc == 0), stop=(dc == 7),
                r=["rw", ("h1T", b)], w=[lk])
        L = lg[b4]
        Lk = ("lg", b4)
        S = st[b]
        Sk = ("st", b)
        R_ = rt[b4]
        Rk = ("rt", b4)
        P.c("dve", "tensor_tensor", out=L[:], in0=lps[:, 0:36], in1=rbias[:], op=ALU.add, r=[lk, "rbias"], w=[Lk])
        P.c("dve", "tensor_reduce", out=S[:, 5:6], in_=L[:, 0:4], axis=AX.X, op=ALU.max, r=[Lk], w=[Sk])
        P.c("dve", "tensor_scalar", out=S[:, 6:7], in0=S[:, 5:6], scalar1=-1.0, scalar2=None, op0=ALU.mult,
            r=[Sk], w=[Sk])
        P.c("act", "activation", out=R_[:, 0, 0:4], in_=L[:, 0:4], func=AF.Exp, bias=S[:, 6:7], scale=1.0,
            r=[Lk, Sk], w=[Rk])
        P.c("dve", "tensor_reduce", out=S[:, 7:8], in_=R_[:, 0, 0:4], axis=AX.X, op=ALU.add, r=[Rk], w=[Sk])
        P.c("dve", "reciprocal", out=S[:, 7:8], in_=S[:, 7:8], r=[Sk], w=[Sk])
        P.c("dve", "tensor_scalar", out=R_[:, 0, 4:8], in0=L[:, 0:4], scalar1=S[:, 5:6], scalar2=None,
            op0=ALU.is_ge, r=[Lk, Sk], w=[Rk])
        P.c("dve", "tensor_scalar", out=R_[:, 0, 8:12], in0=R_[:, 0, 4:8], scalar1=1e9, scalar2=-1e9,
            op0=ALU.mult, op1=ALU.add, r=[Rk], w=[Rk])
        P.c("dve", "tensor_tensor", out=R_[:, 1, :].rearrange("p (g e) -> p g e", g=4),
            in0=L[:, 4:36].rearrange("p (g e) -> p g e", g=4),
            in1=R_[:, 0, 8:12].unsqueeze(2).to_broadcast([128, 4, 8]), op=ALU.add, r=[Lk, Rk], w=[Rk])
        P.c("dve", "tensor_reduce", out=S[:, 8:9], in_=R_[:, 1, :], axis=AX.X, op=ALU.max, r=[Rk], w=[Sk])
        P.c("dve", "tensor_scalar", out=R_[:, 2, :], in0=R_[:, 1, :], scalar1=S[:, 8:9], scalar2=None,
            op0=ALU.is_ge, r=[Rk, Sk], w=[Rk])
        P.c("dve", "scalar_tensor_tensor", out=R_[:, 1, :], in0=R_[:, 2, :], scalar=-1e9, in1=R_[:, 1, :],
            op0=ALU.mult, op1=ALU.add, r=[Rk], w=[Rk])
        P.c("dve", "tensor_reduce", out=S[:, 9:10], in_=R_[:, 1, :], axis=AX.X, op=ALU.max, r=[Rk], w=[Sk])
        P.c("dve", "tensor_scalar", out=R_[:, 3, :], in0=R_[:, 1, :], scalar1=S[:, 9:10], scalar2=None,
            op0=ALU.is_ge, r=[Rk, Sk], w=[Rk])
        P.c("dve", "tensor_tensor", out=S[:, 10:11], in0=S[:, 9:10], in1=S[:, 8:9], op=ALU.subtract,
            r=[Sk], w=[Sk])
        P.c("act", "activation", out=S[:, 11:12], in_=S[:, 10:11], func=AF.Exp, r=[Sk], w=[Sk])
        P.c("dve", "tensor_scalar", out=S[:, 12:13], in0=S[:, 11:12], scalar1=1.0, scalar2=None, op0=ALU.add,
            r=[Sk], w=[Sk])
        P.c("dve", "reciprocal", out=S[:, 12:13], in_=S[:, 12:13], r=[Sk], w=[Sk])
        P.c("dve", "tensor_tensor", out=S[:, 13:14], in0=S[:, 11:12], in1=S[:, 12:13], op=ALU.mult,
            r=[Sk], w=[Sk])
        P.c("dve", "tensor_scalar", out=S[:, 12:14], in0=S[:, 12:14], scalar1=S[:, 7:8], scalar2=None,
            op0=ALU.mult, r=[Sk], w=[Sk])
        P.c("dve", "tensor_copy", out=gk_all[:, i, :], in_=S[:, 12:14], r=[Sk], w=[("gk", i)])
    def tileY(i):
        b = i % 2
        b4 = i % 4
        L = lg[b4]
        Lk = ("lg", b4)
        S = st[b]
        Sk = ("st", b)
        R_ = rt[b4]
        Rk = ("rt", b4)
        ob = ohb[b]
        obk = ("ohb", b)
        P.c("dve", "tensor_tensor", out=R_[:, 0, :], in0=R_[:, 2, :], in1=R_[:, 3, :], op=ALU.add, r=[Rk], w=[Rk])
        P.c("dve", "tensor_copy", out=ob[:], in_=R_[:, 0, :], r=[Rk], w=[obk])
        rps, rk_ = cur_rr[0].next()
        P.c("pe", "matmul", rps[:, 0:32], rcb[:, 0, :], ob[:], start=True, stop=False, r=["rcb", obk], w=[rk_])
        P.c("pe", "matmul", rps[:, 0:32], rcb[:, 1, :], ohsb[:], start=False, stop=True, r=["rcb", "ohsb"], w=[rk_])
        P.c("dve", "tensor_copy", out=R_[:, 1, :], in_=rps[:, 0:32], r=[rk_], w=[Rk])
        P.c("dve", "tensor_scalar", out=L[:, 0:32], in0=R_[:, 1, :], scalar1=float(CAP), scalar2=1e6,
            op0=ALU.is_ge, op1=ALU.mult, r=[Rk], w=[Lk])
        P.c("dve", "tensor_tensor", out=R_[:, 1, :], in0=R_[:, 1, :], in1=L[:, 0:32], op=ALU.add, r=[Rk, Lk], w=[Rk])
        P.c("dve", "tensor_tensor", out=R_[:, 1, :], in0=R_[:, 1, :], in1=rconst[:, 2, 0:32], op=ALU.add,
            r=[Rk, "rconst"], w=[Rk])
        df = dest_f[b]
        dfk = ("dest_f", b)
        for k_ in range(2):
            P.c("dve", "tensor_tensor", out=L[:, 0:32], in0=R_[:, 2 + k_, :], in1=R_[:, 1, :], op=ALU.mult,
                r=[Rk, Lk], w=[Lk])
            P.c("dve", "tensor_reduce", out=df[:, k_:k_ + 1], in_=L[:, 0:32], axis=AX.X, op=ALU.add,
                r=[Lk], w=[dfk])
        P.c("dve", "tensor_copy", out=dest_i[:, 2 * i:2 * i + 2], in_=df[:], r=[dfk], w=[("dest", i)])
        P.c("dve", "tensor_tensor", out=ohsum[:], in0=ohsum[:], in1=R_[:, 0, :], op=ALU.add, r=["ohsum", Rk], w=["ohsum"])
        P.c("dve", "tensor_copy", out=ohsb[:], in_=ohsum[:], r=["ohsum"], w=["ohsb"])
        for k_ in range(2):
            P.d("pool", "indirect_dma_start", out=xbuf[:, :],
                out_offset=bass.IndirectOffsetOnAxis(ap=dest_i[:, 2 * i + k_:2 * i + k_ + 1], axis=0),
                in_=h1b[b4][:, :], in_offset=None, bounds_check=bc_reg, oob_is_err=False,
                r=[("h1b", b4), ("dest", i)], w=[("xbuf", i, k_)])
    for i in range(0, 16, 2):
        streams = []
        for ii, rr_ in ((i, rrA), (i + 1, rrB)):
            cur_rr[0] = rr_
            P.capture()
            tileX(ii)
            streams.append(P.end_capture())
        if i > 0:
            cur_rr[0] = rrG
            P.capture()
            tileY(i - 2)
            tileY(i - 1)
            streams.append(P.end_capture())
        P.replay_interleaved(streams)
        precast(2)
    cur_rr[0] = ps_rr
    precast(3 * NEXP)
    tileY(14)
    tileY(15)
    P.flush()
    ph.close()

    ph = ExitStack()
    wg = [sb("wg%d" % i, [128, 8, 512], BF16, ph) for i in range(3)]
    wu = [sb("wu%d" % i, [128, 8, 512], BF16, ph) for i in range(3)]
    wd = [sb("wd%d" % i, [128, 4, 1024], BF16, ph) for i in range(3)]
    xg = [sb("xg%d" % i, [128, 2, 1024], BF16, ph) for i in range(2)]
    XT = [sb("XT%d" % i, [128, 8, CAP], BF16, ph) for i in range(2)]
    hidT = [sb("hidT%d" % i, [128, 4, CAP], BF16, ph) for i in range(2)]
    sg = [sb("sg%d" % i, [128, CAP], F32, ph) for i in range(2)]
    ysb = [sb("ysb%d" % i, [128, 2, 1024], BF16, ph) for i in range(2)]
    sgi = 0
    evi = 0

    def loads_x(e):
        wb = e % 2
        P.d("pool", "dma_start", out=xg[wb][:], in_=xbuf[e * CAP:(e + 1) * CAP, :].rearrange("(k p) d -> p k d", p=128),
            w=[("xg", wb)])

    def loads_w(e):
        w3 = e % 3
        P.d("sp", "dma_start", out=wg[w3][:].rearrange("p c f -> p (c f)"), in_=wgb[e], w=[("wg", w3)])
        P.d("sp", "dma_start", out=wu[w3][:].rearrange("p c f -> p (c f)"), in_=wub[e], w=[("wu", w3)])
        P.d("sp", "dma_start", out=wd[w3][:].rearrange("p c f -> p (c f)"), in_=wdb[e], w=[("wd", w3)])

    def prep(e):
        nonlocal evi
        wb = e % 2
        for blk in range(2):
            for hq in range(2):
                tps_, tpk_ = ps_rr.next()
                for d4 in range(4):
                    dc = hq * 4 + d4
                    P.c("pe", "matmul", tps_[:, d4 * 128:(d4 + 1) * 128], xg[wb][:, blk, dc * 128:(dc + 1) * 128],
                        ident[:], start=True, stop=True, r=["ident", ("xg", wb)], w=[tpk_])
                evi += 1
                if evi % 2:
                    P.c("act", "copy", out=XT[wb][:, hq * 4:hq * 4 + 4, blk * 128:(blk + 1) * 128],
                        in_=tps_[:, 0:512].rearrange("p (c t) -> p c t", c=4), r=[tpk_], w=[("XT", wb)])
                else:
                    P.c("dve", "tensor_copy", out=XT[wb][:, hq * 4:hq * 4 + 4, blk * 128:(blk + 1) * 128],
                        in_=tps_[:, 0:512].rearrange("p (c t) -> p c t", c=4), r=[tpk_], w=[("XT", wb)])

    loads_x(0)
    loads_w(0)
    loads_w(1)
    prep(0)
    for e in range(NEXP):
        wb = e % 2
        w3 = e % 3
        if e + 1 < NEXP:
            loads_x(e + 1)
        if e + 2 < NEXP:
            loads_w(e + 2)
        for ft in range(4):
            fs = slice(ft * 128, (ft + 1) * 128)
            gps, gk = ps_rr.next()
            for dc in range(8):
                P.c("pe", "matmul", gps[:, 0:CAP], wg[w3][:, dc, fs], XT[wb][:, dc, :],
                    start=(dc == 0), stop=(dc == 7), r=[("wg", w3), ("XT", wb)], w=[gk])
            for dc in range(8):
                P.c("pe", "matmul", gps[:, CAP:2 * CAP], wu[w3][:, dc, fs], XT[wb][:, dc, :],
                    start=(dc == 0), stop=(dc == 7), r=[("wu", w3), ("XT", wb)], w=[gk])
            s_ = sg[sgi % 2]
            sk_ = ("sg", sgi % 2)
            sgi += 1
            P.c("act", "activation", out=s_[:], in_=gps[:, 0:CAP], func=AF.Silu, r=[gk], w=[sk_])
            P.c("dve", "tensor_tensor", out=hidT[wb][:, ft, :], in0=gps[:, CAP:2 * CAP], in1=s_[:], op=ALU.mult,
                r=[gk, sk_], w=[("hidT", wb)])
        if e + 1 < NEXP:
            prep(e + 1)
        for blk in range(2):
            for half in range(2):
                cs_ = slice(half * 512, (half + 1) * 512)
                yps, yk = ps_rr.next()
                for fc in range(4):
                    P.c("pe", "matmul", yps[:, 0:512], hidT[wb][:, fc, blk * 128:(blk + 1) * 128],
                        wd[w3][:, fc, cs_], start=(fc == 0), stop=(fc == 3),
                        r=[("hidT", wb), ("wd", w3)], w=[yk])
                evi += 1
                if evi % 2:
                    P.c("act", "copy", out=ysb[wb][:, blk, cs_], in_=yps[:, 0:512], r=[yk], w=[("ysb", wb)])
                else:
                    P.c("dve", "tensor_copy", out=ysb[wb][:, blk, cs_], in_=yps[:, 0:512], r=[yk], w=[("ysb", wb)])
        P.d("pool", "dma_start", out=ybuf[e * CAP:(e + 1) * CAP, :].rearrange("(k p) d -> p k d", p=128),
            in_=ysb[wb][:], r=[("ysb", wb)], w=[("ybuf", e)])
    P.flush()
    ph.close()

    ph = ExitStack()
    ln2g = sb("ln2g", [128, 1024], F32, ph)
    ln2b = sb("ln2b", [128, 1024], F32, ph)
    epsc2 = sb("epsc2b", [128, 1], F32, ph)
    scr = [sb("scrE%d" % i, [128, 1024], F32, ph) for i in range(4)]
    ot = [sb("ot%d" % i, [128, 1024], F32, ph) for i in range(4)]
    st = [sb("stE%d" % i, [128, 16], F32, ph) for i in range(4)]
    yg = [sb("yg%d" % i, [128, 2, 1024], BF16, ph) for i in range(4)]
    ac = [sb("ac%d" % i, [128, 1024], F32, ph) for i in range(4)]
    P.d("sp", "dma_start", out=ln2g[:], in_=ln2g_d, w=["ln2g"])
    P.d("sp", "dma_start", out=ln2b[:], in_=ln2b_d, w=["ln2b"])
    P.c("dve", "memset", epsc2[:], EPS, w=["epsc2"])
    def tileE(i):
        b = i % 4
        y_ = yg[i % 4]
        yk_ = ("yg", i % 4)
        a_ = ac[i % 4]
        ak_ = ("ac", i % 4)
        P.d("sp", "dma_start", out=a_[:], in_=h1buf[i * 128:(i + 1) * 128, :], w=[ak_])
        P.c("pool", "memset", y_[:], 0.0, w=[yk_])
        for k_ in range(2):
            P.d("pool", "indirect_dma_start", out=y_[:, k_, :], out_offset=None, in_=ybuf[:, :],
                in_offset=bass.IndirectOffsetOnAxis(ap=dest_i[:, 2 * i + k_:2 * i + k_ + 1], axis=0),
                bounds_check=bc_reg, oob_is_err=False, r=[yk_], w=[yk_])
        for k_ in range(2):
            P.c("dve", "scalar_tensor_tensor", out=a_[:], in0=y_[:, k_, :], scalar=gk_all[:, i, k_:k_ + 1],
                in1=a_[:], op0=ALU.mult, op1=ALU.add, r=[yk_, ak_], w=[ak_])
        layer_norm(b, a_[:], ak_, ot[b][:], ("ot", b), ln2g[:], "ln2g", ln2b[:], "ln2b",
                   st[b], ("stE", b), scr[b][:], ("scrE", b))
        P.d("sp", "dma_start", out=out_d[i * 128:(i + 1) * 128, :], in_=ot[b][:], r=[("ot", b)], w=[("out", i)])

    for i in range(0, 16, 4):
        streams = []
        for ii in range(i, i + 4):
            P.capture()
            tileE(ii)
            streams.append(P.end_capture())
        P.replay_interleaved(streams)
    P.flush()
    ph.close()
    phC.close()

    P.skip = False
    stats = P.stats
    return nc, es, stats


def host_consts():
    j = np.arange(128)[:, None].astype(np.float64)
    i = np.arange(128)[None, :].astype(np.float64)
    masks = np.zeros((128, 24, 256), np.float32)
    for ci, (w, r) in enumerate(DIL_CFG):
        for h in range(8):
            s = 2.0 ** (-(h + 1))
            rel_p = i + 128 - j
            rel_c = i - j
            masks[:, ci * 8 + h, 0:128] = np.where(i <= j, np.exp(-s * r * rel_p), 0.0)
            masks[:, ci * 8 + h, 128:256] = np.where(i >= j, np.exp(-s * r * np.maximum(rel_c, 0)), 0.0)
    wn = np.full((65, 64), 1.0 / 64, np.float32)
    wn[64, :] = EPS
    sI = np.arange(128)[:, None]
    tI = np.arange(128)[None, :]
    cmat = np.zeros((128, 4, 128), np.float32)
    cmat[:, 0, :] = np.where(sI <= tI, -1.0 / 16, 0.0)
    cmat[:, 1, :] = np.where(sI > tI, -1.0 / 16, 0.0)
    cmat[:, 2, :] = np.where(sI <= tI, 1.0, 0.0)
    cmat[:, 3, :] = -1.0 / 16
    rconst = np.zeros((128, 3, 128), np.float32)
    rconst[:, 0, :] = np.where(sI < tI, 1.0, 0.0)
    rconst[:, 1, :] = 1.0
    rconst[:, 2, 0:32] = (np.arange(32) * CAP)[None, :]
    return dict(masks=masks, ident=np.eye(128, dtype=np.float32), wn=wn, cmat=cmat, rconst=rconst)


def make_in_maps(inputs):
    x = np.asarray(inputs["x"], np.float32)
    w_in = np.asarray(inputs["w_in"], np.float32)[0]
    cs = host_consts()
    w_inA = np.concatenate([w_in[:, 1552:2064], w_in[:, 2064:2576], w_in[:, 2576:3088]], axis=1)
    w_inA = np.ascontiguousarray(w_inA.reshape(8, 128, 1536).transpose(1, 0, 2))
    gdil = np.asarray(inputs["dil_norm_g"], np.float32)[0].reshape(64, 1)
    w_inB = np.ascontiguousarray(w_in[:, 0:1552].reshape(8, 128, 1552).transpose(1, 0, 2))
    w2aug = np.concatenate([np.asarray(inputs["gla_gate_w2"], np.float32)[0],
                            np.asarray(inputs["gla_gate_b"], np.float32)[0][None, :]], axis=0)
    gnorm = np.ascontiguousarray(np.broadcast_to(np.asarray(inputs["gla_norm_g"], np.float32)[0][None, :], (128, 128)))
    w_out = np.asarray(inputs["w_out"], np.float32)[0]
    w_og = np.ascontiguousarray(w_out[0:512].reshape(4, 128, 1024).transpose(1, 0, 2))
    w_od = np.ascontiguousarray(w_out[512:1024].reshape(8, 64, 1024).transpose(1, 0, 2))
    rwm = np.concatenate([np.asarray(inputs["router_coarse_w"], np.float32)[0],
                          np.asarray(inputs["router_fine_w"], np.float32)[0].reshape(1024, 32)], axis=1)
    rwm = np.ascontiguousarray(rwm.reshape(8, 128, 36).transpose(1, 0, 2))
    rb = np.concatenate([np.asarray(inputs["router_coarse_b"], np.float32)[0],
                         np.asarray(inputs["router_fine_b"], np.float32)[0].reshape(32)])
    rbias = np.ascontiguousarray(np.broadcast_to(rb[None, :], (128, 36)))

    def bc(name):
        return np.ascontiguousarray(np.broadcast_to(np.asarray(inputs[name], np.float32)[0][None, :], (128, 1024)))

    wgm = np.ascontiguousarray(np.asarray(inputs["expert_w_gate"], np.float32)[0].reshape(NEXP, 8, 128, 512).transpose(0, 2, 1, 3))
    wum = np.ascontiguousarray(np.asarray(inputs["expert_w_up"], np.float32)[0].reshape(NEXP, 8, 128, 512).transpose(0, 2, 1, 3))
    wdm = np.ascontiguousarray(np.asarray(inputs["expert_w_down"], np.float32)[0].reshape(NEXP, 4, 128, 1024).transpose(0, 2, 1, 3))
    shared = dict(w_og=w_og, w_od=w_od, rw=rwm, rbias=rbias, ln1g=bc("ln1_g"), ln1b=bc("ln1_b"),
                  ln2g=bc("ln2_g"), ln2b=bc("ln2_b"), wg=wgm, wu=wum, wd=wdm, rconst=cs["rconst"])
    maps = []
    for c in range(NCORES):
        b, half = c // 2, c % 2
        xT = np.zeros((1024, TALL), np.float32)
        if half == 1:
            xT[:, :TOWN] = x[b, :TOWN].T
        xT[:, TOWN:] = x[b, half * TOWN:(half + 1) * TOWN].T
        maps.append(dict(xT=xT, w_inA=w_inA, masks=cs["masks"], ident=cs["ident"], wn=cs["wn"],
                         gdil=gdil, pv=np.full((128, 1), float(half), np.float32),
                         w_inB=w_inB, w2aug=w2aug, gnorm=gnorm, cmat=cs["cmat"],
                         xtok=np.ascontiguousarray(x[b, half * TOWN:(half + 1) * TOWN]), **shared))
    return maps


def kernel(**inputs):
    nc, es, stats = build_program()
    with es:
        res = run_bass_kernel_spmd(nc, make_in_maps(inputs), core_ids=list(range(NCORES)))
    out = np.zeros((4, 2 * TOWN, 1024), np.float32)
    for c in range(NCORES):
        out[c // 2, (c % 2) * TOWN:(c % 2 + 1) * TOWN] = res.results[c]["out"]
    return out
```

```python
import numpy as np
from contextlib import ExitStack
import concourse.bass as bass
import concourse.mybir as mybir
from concourse.bass_utils import run_bass_kernel_spmd

F32, BF16, I32 = mybir.dt.float32, mybir.dt.bfloat16, mybir.dt.int32
AF = mybir.ActivationFunctionType
ALU = mybir.AluOpType
AX = mybir.AxisListType

NCORES = 8
TOWN = 2048
TALL = 4096
DIL_CFG = ((128, 1), (512, 4), (2048, 16))
EPS = 1e-5
ALPHA = 2.0 ** 0.25
NEXP = 32
CAP = 256
NROWS = NEXP * CAP


class Prog:
    COMPUTE = ("pe", "act", "dve", "pool")

    def __init__(self, nc, es, n_dma_sems=32):
        self.nc = nc
        self.h = {"pe": nc.tensor, "act": nc.scalar, "dve": nc.vector,
                  "pool": nc.gpsimd, "sp": nc.sync}
        self.sem = {e: es.enter_context(nc.semaphore("s_" + e)) for e in self.COMPUTE}
        self.dsem = [es.enter_context(nc.semaphore("d%d" % i)) for i in range(n_dma_sems)]
        self.ops = []
        self.res = {}

    def _deps(self, opid, rk, reads, writes):
        deps = set()
        for k in reads:
            r = self.res.get(k)
            if r is not None and r[0] is not None:
                deps.add(r[0])
        for k in writes:
            r = self.res.get(k)
            if r is not None:
                if r[0] is not None:
                    deps.add(r[0])
                deps.update(r[1].values())
        for k in reads:
            r = self.res.setdefault(k, [None, {}])
            r[1][rk] = opid
        for k in writes:
            self.res[k] = [opid, {}]
        deps.discard(opid)
        return deps

    def capture(self):
        self._cap = []

    def end_capture(self):
        L, self._cap = self._cap, None
        return L

    def replay_interleaved(self, lists):
        lists = [list(L) for L in lists]
        while any(lists):
            for L in lists:
                if L:
                    kind, eng, name, args, kw, r, w = L.pop(0)
                    (self.c if kind == "c" else self.d)(eng, name, *args, r=r, w=w, **kw)

    def c(self, eng, name, *args, r=(), w=(), **kw):
        if getattr(self, "_cap", None) is not None:
            self._cap.append(("c", eng, name, args, kw, r, w))
            return None
        if getattr(self, "skip", False):
            return None
        opid = len(self.ops)
        deps = self._deps(opid, eng, r, w)
        self.ops.append(dict(kind="c", eng=eng, fn=(name, args, kw), deps=deps))
        return opid

    def d(self, q, name, *args, r=(), w=(), **kw):
        if getattr(self, "_cap", None) is not None:
            self._cap.append(("d", q, name, args, kw, r, w))
            return None
        if getattr(self, "skip", False):
            return None
        opid = len(self.ops)
        deps = self._deps(opid, ("dma", opid), r, w)
        self.ops.append(dict(kind="d", eng=q, fn=(name, args, kw), deps=deps))
        return opid

    def flush(self):
        if not hasattr(self, "cnt"):
            self.cnt = {e: 0 for e in self.COMPUTE}
            self.rank = {}
            self.waited = {e: {} for e in self.h}
            self.dcnt = [0] * len(self.dsem)
            self.rr = 0
            self.rr_sw = 0
            self.nwait = 0
            self.done = 0
        ops = self.ops
        lo = self.done
        needed = set()
        last = {}
        for i in range(lo, len(ops)):
            op = ops[i]
            if op["kind"] == "c":
                last[op["eng"]] = i
            for d in op["deps"]:
                if d < lo:
                    continue
                dop = ops[d]
                if dop["kind"] == "c":
                    if dop["eng"] == "pe" and op["kind"] == "c" and op["eng"] == "pe":
                        continue
                    needed.add(d)
        needed.update(last.values())
        cnt, rank, waited, dcnt = self.cnt, self.rank, self.waited, self.dcnt
        for i in range(lo, len(ops)):
            op = ops[i]
            q = op["eng"]
            need = {}
            for d in op["deps"]:
                if d < lo:
                    continue
                dop = ops[d]
                if dop["kind"] == "c":
                    if dop["eng"] == "pe" and op["kind"] == "c" and q == "pe":
                        continue
                    sk, val = dop["eng"], rank[d]
                else:
                    sk, val = dop["dsem"], dop["dval"]
                if waited[q].get(sk, 0) < val:
                    need[sk] = max(need.get(sk, 0), val)
            if op["kind"] == "d":
                half = len(self.dsem) // 2
                if q == "pool":
                    s = self.rr_sw
                    self.rr_sw = (self.rr_sw + 1) % half
                else:
                    s = half + self.rr
                    self.rr = (self.rr + 1) % half
                sk = ("d", s)
                if dcnt[s] > 0 and waited[q].get(sk, 0) < dcnt[s]:
                    need[sk] = max(need.get(sk, 0), dcnt[s])
            for sk, val in need.items():
                sh = self.sem[sk] if isinstance(sk, str) else self.dsem[sk[1]]
                self.h[q].wait_ge(sh, val)
                waited[q][sk] = val
                self.nwait += 1
            name, args, kw = op["fn"]
            ins = getattr(self.h[q], name)(*args, **kw)
            if op["kind"] == "c":
                if i in needed:
                    cnt[q] += 1
                    rank[i] = cnt[q]
                    ins.then_inc(self.sem[q], 1)
            else:
                dcnt[s] += 16
                ins.then_inc(self.dsem[s], 16)
                op["dsem"] = ("d", s)
                op["dval"] = dcnt[s]
        self.done = len(ops)
        for q in self.h:
            for e in self.COMPUTE:
                if cnt[e] > waited[q].get(e, 0):
                    self.h[q].wait_ge(self.sem[e], cnt[e])
                    waited[q][e] = cnt[e]
            for s_, v in enumerate(dcnt):
                if v > waited[q].get(("d", s_), 0):
                    self.h[q].wait_ge(self.dsem[s_], v)
                    waited[q][("d", s_)] = v
        self.res = {}
        self.stats = dict(n_ops=len(ops), n_wait=self.nwait, cnt=dict(cnt), n_dma=sum(dcnt) // 16)
        return self.stats


class RR:
    def __init__(self, items):
        self.items = list(items)
        self.i = 0

    def next(self):
        it = self.items[self.i]
        self.i = (self.i + 1) % len(self.items)
        return it


def build_program(dbg=()):
    nc = bass.Bass("TRN2", target_bir_lowering=False)
    es = ExitStack()
    P = Prog(nc, es)

    def din(name, shape, dt=F32):
        return nc.dram_tensor(name, list(shape), dt, kind="ExternalInput").ap()

    def dout(name, shape, dt=F32):
        return nc.dram_tensor(name, list(shape), dt, kind="ExternalOutput").ap()

    uid = [0]

    def sb(name, shape, dt, st=None):
        uid[0] += 1
        return (st or es).enter_context(nc.sbuf_tensor("sb%d_%s" % (uid[0], name), list(shape), dt))

    xT_d = din("xT", [1024, TALL])
    w_inA_d = din("w_inA", [128, 8, 1536])
    masks_d = din("masks", [128, 24, 256])
    ident_d = din("ident", [128, 128])
    wn_d = din("wn", [65, 64])
    gdil_d = din("gdil", [64, 1])
    pv_d = din("pv", [128, 1])
    w_inB_d = din("w_inB", [128, 8, 1552])
    w2aug_d = din("w2aug", [17, 256])
    gnorm_d = din("gnorm", [128, 128])
    cmat_d = din("cmat", [128, 4, 128])
    w_og_d = din("w_og", [128, 4, 1024])
    w_od_d = din("w_od", [64, 8, 1024])
    rw_d = din("rw", [128, 8, 36])
    rbias_d = din("rbias", [128, 36])
    ln1g_d = din("ln1g", [128, 1024])
    ln1b_d = din("ln1b", [128, 1024])
    ln2g_d = din("ln2g", [128, 1024])
    ln2b_d = din("ln2b", [128, 1024])
    xtok_d = din("xtok", [TOWN, 1024])
    wg_d = din("wg", [NEXP, 128, 8, 512])
    wu_d = din("wu", [NEXP, 128, 8, 512])
    wd_d = din("wd", [NEXP, 128, 4, 1024])
    out_d = dout("out", [TOWN, 1024])
    rconst_d = din("rconst", [128, 3, 128])
    xbuf = nc.dram_tensor("xbuf", [NROWS, 1024], BF16, kind="Internal").ap()
    ybuf = nc.dram_tensor("ybuf", [NROWS, 1024], BF16, kind="Internal").ap()
    h1buf = nc.dram_tensor("h1buf", [TOWN, 1024], F32, kind="Internal").ap()
    wgb = nc.dram_tensor("wgb", [NEXP, 128, 4096], BF16, kind="Internal").ap()
    wub = nc.dram_tensor("wub", [NEXP, 128, 4096], BF16, kind="Internal").ap()
    wdb = nc.dram_tensor("wdb", [NEXP, 128, 4096], BF16, kind="Internal").ap()
    pc_i = [0]

    def precast(n=1):
        for _ in range(n):
            k = pc_i[0]
            if k >= 3 * NEXP:
                return
            pc_i[0] += 1
            e_, m_ = k // 3, k % 3
            src = (wg_d, wu_d, wd_d)[m_][e_].rearrange("p c f -> p (c f)")
            dst = (wgb, wub, wdb)[m_][e_]
            P.d("pool", "dma_start", out=dst, in_=src, w=[("wb", k)])
    bc_reg = nc.gpsimd.to_reg(NROWS - 1)
    if "odil" in dbg:
        odil_o = dout("odil_dbg", [64, 8, TOWN])
    if "ogla" in dbg:
        ogla_o = dout("ogla_dbg", [128, 4, TOWN])

    ident = sb("ident", [128, 128], BF16)
    wn = sb("wn", [65, 64], BF16)
    gdil = sb("gdil", [64, 1], F32)
    pvt = sb("pvt", [128, 1], F32)
    o_dilT = sb("o_dilT", [64, 8, TOWN], BF16)
    o_glaT = sb("o_glaT", [128, 4, TOWN], BF16)

    ps = [es.enter_context(nc.psum_tensor("ps%d" % i, [128, 512], F32)) for i in range(7)]
    psb = es.enter_context(nc.psum_tensor("psb", [128, 1024], BF16))
    ps_rr = RR([(ps[i], ("ps", i)) for i in range(7)])

    P.d("pool", "dma_start", out=ident[:], in_=ident_d, w=["ident"])
    P.d("pool", "dma_start", out=wn[:], in_=wn_d, w=["wn"])
    P.d("sp", "dma_start", out=gdil[:], in_=gdil_d, w=["gdil"])
    P.d("sp", "dma_start", out=pvt[:], in_=pv_d, w=["pvt"])
    P.flush()

    P.skip = "skipA" in dbg
    ph = ExitStack()
    xs = [sb("xs%d" % i, [128, 8, 512], BF16, ph) for i in range(2)]
    w_inB = sb("w_inB", [128, 8, 1552], BF16, ph)
    w2aug = sb("w2aug", [17, 256], BF16, ph)
    gnorm = sb("gnorm", [128, 128], F32, ph)
    cmat = sb("cmat", [128, 4, 128], F32, ph)
    causal_bf = sb("causal_bf", [128, 128], BF16, ph)
    epsc = sb("epsc", [128, 1], F32, ph)
    gaTs = [sb("gaT%d" % i, [32, 512], BF16, ph) for i in range(2)]
    qkTs = [sb("qkT%d" % i, [128, 4, 512], F32, ph) for i in range(2)]
    Sst = sb("Sst", [128, 2, 128], F32, ph)
    Sbf = [sb("Sbf%d" % i, [128, 2, 128], BF16, ph) for i in range(2)]
    NB = 4
    e1 = [sb("e1_%d" % i, [128, 256], F32, ph) for i in range(NB)]
    lap = [sb("lap%d" % i, [128, 256], F32, ph) for i in range(NB)]
    lhl = [sb("lhl%d" % i, [128, 2, 256], BF16, ph) for i in range(NB)]
    cmb = sb("cmb", [128, 4, 128], BF16, ph)
    dec = [sb("dec%d" % i, [128, 2], F32, ph) for i in range(NB)]
    ek = [sb("ek%d" % i, [128, 256], F32, ph) for i in range(NB)]
    khat = [sb("khat%d" % i, [128, 256], BF16, ph) for i in range(NB)]
    vbf = [sb("vbf%d" % i, [128, 512], BF16, ph) for i in range(NB)]
    gs = [sb("gs%d" % i, [128, 512], F32, ph) for i in range(NB)]
    ebT = [sb("ebT%d" % i, [128, 2, 128], F32, ph) for i in range(NB)]
    enbT = [sb("enbT%d" % i, [128, 2, 128], F32, ph) for i in range(NB)]
    qtl = [sb("qtl%d" % i, [128, 2, 2, 128], BF16, ph) for i in range(NB)]
    ktl = [sb("ktl%d" % i, [128, 2, 128], BF16, ph) for i in range(NB)]
    Am = [sb("Am%d" % i, [128, 4, 128], BF16, ph) for i in range(NB)]
    sqo = [sb("sqo%d" % i, [128, 512], F32, ph) for i in range(NB)]
    ssr = [sb("ssr%d" % i, [128, 4], F32, ph) for i in range(NB)]
    onrm = [sb("onrm%d" % i, [128, 512], F32, ph) for i in range(NB)]
    ogt = [sb("ogt%d" % i, [128, 512], BF16, ph) for i in range(NB)]
    zt = sb("zt", [128, 8192], BF16, ph)
    P.c("pool", "memset", zt[:], 0.0, w=["zt"])
    xz = xbuf.rearrange("(c p j) d -> c p (j d)", p=128, j=8)
    for c_ in range(NROWS // 1024):
        P.d("sp", "dma_start", out=xz[c_], in_=zt[:], r=["zt"], w=[("xbufz", c_)])
    P.d("pool", "dma_start", out=w_inB[:], in_=w_inB_d, w=["w_inB"])
    P.d("pool", "dma_start", out=w2aug[:], in_=w2aug_d, w=["w2aug"])
    P.d("sp", "dma_start", out=gnorm[:], in_=gnorm_d, w=["gnorm"])
    P.d("sp", "dma_start", out=cmat[:], in_=cmat_d, w=["cmat"])
    P.c("dve", "tensor_copy", out=causal_bf[:], in_=cmat[:, 2, :], r=["cmat"], w=["causal_bf"])
    P.c("dve", "tensor_copy", out=cmb[:], in_=cmat[:], r=["cmat"], w=["cmb"])
    P.c("dve", "memset", epsc[:], EPS, w=["epsc"])
    for i in range(2):
        P.c("dve", "memset", gaTs[i][:], 1.0, w=[("gaT", i)])
    P.c("dve", "memset", Sst[:], 0.0, w=["Sst"])
    for i in range(2):
        P.c("dve", "memset", Sbf[i][:], 0.0, w=[("Sbf", i)])
    for i in range(NB):
        P.c("pool", "memset", qtl[i][:], 0.0, w=[("qtl", i)])
    xT_v = xT_d.rearrange("(c p) t -> p c t", p=128)
    TRI, RTRI, CAUS, ONEG = 0, 1, 2, 3
    def group_ops(g):
        own = g >= 4
        xb = xs[g % 2]
        xk = ("xs", g % 2)
        gat = gaTs[g % 2]
        gak = ("gaT", g % 2)
        qk_ = qkTs[g % 2]
        P.d("pool", "dma_start", out=xb[:], in_=xT_v[:, :, g * 512:(g + 1) * 512], w=[xk])
        pst, psk = ps_rr.next()
        for dc in range(8):
            P.c("pe", "matmul", pst[0:16, 0:512], w_inB[:, dc, 1536:1552], xb[:, dc, :],
                start=(dc == 0), stop=(dc == 7), r=["w_inB", xk], w=[psk])
        P.c("act", "copy", out=gat[0:16, :], in_=pst[0:16, 0:512], r=[psk], w=[gak])
        if own:
            for ct in range(4):
                pst, psk = ps_rr.next()
                for dc in range(8):
                    P.c("pe", "matmul", pst[:, 0:512], w_inB[:, dc, ct * 128:(ct + 1) * 128], xb[:, dc, :],
                        start=(dc == 0), stop=(dc == 7), r=["w_inB", xk], w=[psk])
                P.c("act", "copy", out=qk_[:, ct, :], in_=pst[:, 0:512], r=[psk], w=[("qkT", g % 2, ct)])

    def Pst(c):
        g, j = c // 4, c % 4
        own = g >= 4
        xb = xs[g % 2]
        xk = ("xs", g % 2)
        gat = gaTs[g % 2]
        gak = ("gaT", g % 2)
        qk_ = qkTs[g % 2]
        b = c % NB
        tok = slice(j * 128, (j + 1) * 128)
        zps, zk = cur_rr[0].next()
        P.c("pe", "matmul", zps[:, 0:256], gat[0:17, tok], w2aug[0:17, :], start=True, stop=True,
            r=[gak, "w2aug"], w=[zk])
        P.c("act", "activation", out=e1[b][:], in_=zps[:, 0:256], func=AF.Exp, scale=-1.0,
            r=[zk], w=[("e1", b)])
        P.c("act", "activation", out=lap[b][:], in_=e1[b][:], func=AF.Ln, bias=1.0,
            r=[("e1", b)], w=[("lap", b)])
        P.c("dve", "tensor_copy", out=lhl[b][:, 0, :], in_=lap[b][:], r=[("lap", b)], w=[("lhl", b)])
        P.c("dve", "tensor_tensor", out=lhl[b][:, 1, :], in0=lap[b][:], in1=lhl[b][:, 0, :], op=ALU.subtract,
            r=[("lap", b), ("lhl", b)], w=[("lhl", b)])
        dps, dk_ = cur_rr[0].next()
        for T in range(2):
            for hl_ in range(2):
                P.c("pe", "matmul", dps[:, T:T + 1], lhl[b][:, hl_, T * 128:(T + 1) * 128], cmb[:, ONEG, 0:1],
                    start=(hl_ == 0), stop=(hl_ == 1), r=[("lhl", b), "cmb"], w=[dk_])
        for hl_ in range(2):
            P.c("pe", "matmul", dps[:, 256:512], cmb[:, RTRI, :], lhl[b][:, hl_, :],
                start=(hl_ == 0), stop=(hl_ == 1), r=[("lhl", b), "cmb"], w=[dk_])
        P.c("act", "activation", out=dec[b][:], in_=dps[:, 0:2], func=AF.Exp, r=[dk_], w=[("dec", b)])
        P.c("act", "activation", out=ek[b][:], in_=dps[:, 256:512], func=AF.Exp, r=[dk_], w=[("ek", b)])
        kps, kk = cur_rr[0].next()
        for dc in range(8):
            P.c("pe", "matmul", kps[:, 0:256], xb[:, dc, tok], w_inB[:, dc, 256:512],
                start=(dc == 0), stop=(dc == 7), r=["w_inB", xk], w=[kk])
        P.c("dve", "tensor_tensor", out=khat[b][:], in0=kps[:, 0:256], in1=ek[b][:], op=ALU.mult,
            r=[kk, ("ek", b)], w=[("khat", b)])
        vps, vk = cur_rr[0].next()
        for dc in range(8):
            P.c("pe", "matmul", vps[:, 0:512], xb[:, dc, tok], w_inB[:, dc, 512:1024],
                start=(dc == 0), stop=(dc == 7), r=["w_inB", xk], w=[vk])
        P.c("act", "copy", out=vbf[b][:], in_=vps[:, 0:512], r=[vk], w=[("vbf", b)])
        if not own:
            return
        gps, gk = cur_rr[0].next()
        for dc in range(8):
            P.c("pe", "matmul", gps[:, 0:512], xb[:, dc, tok], w_inB[:, dc, 1024:1536],
                start=(dc == 0), stop=(dc == 7), r=["w_inB", xk], w=[gk])
        P.c("act", "activation", out=gs[b][:], in_=gps[:, 0:512], func=AF.Silu, r=[gk], w=[("gs", b)])
        P.c("pool", "tensor_tensor", out=gs[b][:].rearrange("p (h e) -> p h e", h=4),
            in0=gs[b][:].rearrange("p (h e) -> p h e", h=4),
            in1=gnorm[:].unsqueeze(1).to_broadcast([128, 4, 128]), op=ALU.mult,
            r=[("gs", b), "gnorm"], w=[("gs", b)])
        bps, bk = cur_rr[0].next()
        for T in range(2):
            for hl_ in range(2):
                P.c("pe", "matmul", bps[:, T * 128:(T + 1) * 128], lhl[b][:, hl_, T * 128:(T + 1) * 128],
                    cmb[:, TRI, :], start=(hl_ == 0), stop=(hl_ == 1), r=[("lhl", b), "cmb"], w=[bk])
        P.c("act", "activation", out=ebT[b][:].rearrange("p t k -> p (t k)"), in_=bps[:, 0:256],
            func=AF.Exp, r=[bk], w=[("ebT", b)])
        P.c("act", "activation", out=enbT[b][:].rearrange("p t k -> p (t k)"), in_=bps[:, 0:256],
            func=AF.Exp, scale=-1.0, r=[bk], w=[("enbT", b)])
        qkk = [("qkT", g % 2, ct) for ct in range(4)]
        for hh in range(2):
            pr = slice(hh * 64, (hh + 1) * 64)
            P.c("dve", "scalar_tensor_tensor", out=qtl[b][pr, hh, :, :], in0=qk_[pr, 0:2, tok], scalar=0.125,
                in1=ebT[b][pr, :, :], op0=ALU.mult, op1=ALU.mult, r=qkk[0:2] + [("ebT", b)], w=[("qtl", b)])
        P.c("dve", "tensor_tensor", out=ktl[b][:], in0=qk_[:, 2:4, tok], in1=enbT[b][:], op=ALU.mult,
            r=qkk[2:4] + [("enbT", b)], w=[("ktl", b)])
        aps, ak = cur_rr[0].next()
        for h in range(4):
            T, hh = h // 2, h % 2
            P.c("pe", "matmul", aps[:, h * 128:(h + 1) * 128], ktl[b][:, T, :], qtl[b][:, hh, T, :],
                start=True, stop=True, r=[("ktl", b), ("qtl", b)], w=[ak])
        P.c("dve", "tensor_tensor", out=Am[b][:], in0=aps[:, 0:512].rearrange("p (h i) -> p h i", h=4),
            in1=causal_bf[:].unsqueeze(1).to_broadcast([128, 4, 128]), op=ALU.mult,
            r=[ak, "causal_bf"], w=[("Am", b)])

    def Gst(c):
        g, j = c // 4, c % 4
        own = g >= 4
        b = c % NB
        sb_old = c % 2
        sb_new = (c + 1) % 2
        if own:
            t0 = (g - 4) * 512 + j * 128
            ops_, ok_ = cur_rr[0].next()
            for h in range(4):
                T, hh = h // 2, h % 2
                P.c("pe", "matmul", ops_[:, h * 128:(h + 1) * 128], qtl[b][:, hh, T, :],
                    Sbf[sb_old][:, T, :], start=True, stop=False,
                    r=[("qtl", b), ("Sbf", sb_old)], w=[ok_])
                P.c("pe", "matmul", ops_[:, h * 128:(h + 1) * 128], Am[b][:, h, :],
                    vbf[b][:, h * 128:(h + 1) * 128], start=False, stop=True,
                    r=[("Am", b), ("vbf", b)], w=[ok_])
            P.c("act", "activation", out=sqo[b][:], in_=ops_[:, 0:512], func=AF.Square,
                r=[ok_], w=[("sqo", b)])
            P.c("dve", "tensor_reduce", out=ssr[b][:], in_=sqo[b][:].rearrange("p (h e) -> p h e", h=4),
                axis=AX.X, op=ALU.add, r=[("sqo", b)], w=[("ssr", b)])
            P.c("act", "activation", out=ssr[b][:], in_=ssr[b][:], func=AF.Ln, scale=1.0 / 128,
                bias=epsc[:, 0:1], r=[("ssr", b), "epsc"], w=[("ssr", b)])
            P.c("act", "activation", out=ssr[b][:], in_=ssr[b][:], func=AF.Exp, scale=-0.5,
                r=[("ssr", b)], w=[("ssr", b)])
            P.c("dve", "tensor_tensor", out=onrm[b][:].rearrange("p (h e) -> p h e", h=4),
                in0=ops_[:, 0:512].rearrange("p (h e) -> p h e", h=4),
                in1=ssr[b][:].unsqueeze(2).to_broadcast([128, 4, 128]), op=ALU.mult,
                r=[ok_, ("ssr", b)], w=[("onrm", b)])
            P.c("pool", "tensor_tensor", out=ogt[b][:], in0=onrm[b][:], in1=gs[b][:], op=ALU.mult,
                r=[("onrm", b), ("gs", b)], w=[("ogt", b)])
            for h in range(4):
                P.c("pe", "transpose", out=psb[:, h * 128:(h + 1) * 128], in_=ogt[b][:, h * 128:(h + 1) * 128],
                    identity=ident[:], r=["ident", ("ogt", b)], w=["psb"])
            P.c("act", "copy", out=o_glaT[:, :, t0:t0 + 128],
                in_=psb[:, 0:512].rearrange("p (h t) -> p h t", h=4), r=["psb"], w=[("ogla", t0 // 128)])
        for T in range(2):
            sps_, sk_ = cur_rr[0].next()
            P.c("pe", "matmul", sps_[:, 0:256], khat[b][:, T * 128:(T + 1) * 128],
                vbf[b][:, T * 256:(T + 1) * 256], start=True, stop=True,
                r=[("khat", b), ("vbf", b)], w=[sk_])
            for hh in range(2):
                pr = slice(hh * 64, (hh + 1) * 64)
                P.c("dve", "scalar_tensor_tensor", out=Sst[pr, T, :], in0=Sst[pr, T, :],
                    scalar=dec[b][pr, T:T + 1], in1=sps_[pr, hh * 128:(hh + 1) * 128],
                    op0=ALU.mult, op1=ALU.add, r=["Sst", ("dec", b), sk_], w=["Sst"])
        P.c("act", "copy", out=Sbf[sb_new][:], in_=Sst[:], r=["Sst"], w=[("Sbf", sb_new)])

    rrA = RR([(ps[i], ("ps", i)) for i in (0, 1, 2)])
    rrB = RR([(ps[i], ("ps", i)) for i in (3, 4, 5)])
    rrG = RR([(ps[i], ("ps", i)) for i in (6,)])
    cur_rr = [ps_rr]
    NCH = TALL // 128
    for pi in range(NCH // 2):
        c0_ = 2 * pi
        if c0_ % 4 == 0:
            group_ops(c0_ // 4)
        streams = []
        for cc, rr_ in ((c0_, rrA), (c0_ + 1, rrB)):
            cur_rr[0] = rr_
            P.capture()
            Pst(cc)
            streams.append(P.end_capture())
        if pi > 0:
            cur_rr[0] = rrG
            P.capture()
            Gst(c0_ - 2)
            Gst(c0_ - 1)
            streams.append(P.end_capture())
        P.replay_interleaved(streams)
        precast(2)
    cur_rr[0] = ps_rr
    Gst(NCH - 2)
    Gst(NCH - 1)

    if "ogla" in dbg:
        P.d("pool", "dma_start", out=ogla_o, in_=o_glaT[:], r=[("ogla", i) for i in range(16)], w=["ogla_o"])

    P.flush()
    ph.close()

    P.skip = "skipB" in dbg
    ph = ExitStack()
    KT = sb("KT", [128, 4, TALL], BF16, ph)
    VT = sb("VT", [128, 4, TALL], BF16, ph)
    QT = sb("QT", [128, 4, TOWN], BF16, ph)
    ph2 = ExitStack()
    xs = [sb("xs%d" % i, [128, 8, 512], BF16, ph2) for i in range(2)]
    w_inA = sb("w_inA", [128, 8, 1536], BF16, ph2)
    P.d("pool", "dma_start", out=w_inA[:], in_=w_inA_d, w=["w_inA"])
    evq = RR(["act", "dve"])
    for g in range(TALL // 512):
        xb = xs[g % 2]
        P.d("pool", "dma_start", out=xb[:], in_=xT_v[:, :, g * 512:(g + 1) * 512], w=[("xs", g % 2)])
        precast(2)
        tiles = [("K", ct) for ct in range(4)] + [("V", ct) for ct in range(4)]
        if g >= 4:
            tiles += [("Q", ct) for ct in range(4)]
        for (which, ct) in tiles:
            col0 = {"Q": 0, "K": 512, "V": 1024}[which] + ct * 128
            pst, psk = ps_rr.next()
            for dc in range(8):
                P.c("pe", "matmul", pst[:, 0:512], w_inA[:, dc, col0:col0 + 128], xb[:, dc, :],
                    start=(dc == 0), stop=(dc == 7), r=["w_inA", ("xs", g % 2)], w=[psk])
            if which == "Q":
                dst = QT[:, ct, (g - 4) * 512:(g - 3) * 512]
            elif which == "K":
                dst = KT[:, ct, g * 512:(g + 1) * 512]
            else:
                dst = VT[:, ct, g * 512:(g + 1) * 512]
            e = evq.next()
            if e == "act":
                P.c("act", "copy", out=dst, in_=pst[:, 0:512], r=[psk], w=[(which, ct, g)])
            else:
                P.c("dve", "tensor_copy", out=dst, in_=pst[:, 0:512], r=[psk], w=[(which, ct, g)])

    def kv_keys(which, ct, tok0, r):
        g0 = tok0 // 512
        g1 = (tok0 + 127 * r) // 512
        return [(which, ct, g) for g in range(g0, g1 + 1)]

    P.flush()
    ph2.close()
    ph2 = ExitStack()
    masks = sb("masks", [128, 24, 256], BF16, ph2)
    acc = sb("acc", [65, 2, TOWN], F32, ph2)
    sq = sb("sq", [65, TOWN], BF16, ph2)
    rstd = [sb("rstd%d" % i, [64, 512], F32, ph2) for i in range(2)]
    Et = [sb("Et%d" % i, [128, 512], BF16, ph2) for i in range(7)]
    Pt = [sb("Pt%d" % i, [128, 2, 256], BF16, ph2) for i in range(7)]
    vs_own = [sb("vso%d" % i, [128, 2, 65], BF16, ph2) for i in range(18)]
    vs_pre = [sb("vsp%d" % i, [128, 2, 65], BF16, ph2) for i in range(14)]
    P.d("pool", "dma_start", out=masks[:], in_=masks_d, w=["masks"])
    for i, t in enumerate(vs_own):
        P.c("dve", "memset", t[:], 1.0, w=[("vso", i)])
    for i, t in enumerate(vs_pre):
        P.c("dve", "memset", t[:], 0.0, w=[("vsp", i)])
        for hl in range(2):
            P.c("dve", "tensor_copy", out=t[:, hl, 64:65], in_=pvt[:, 0:1], r=["pvt"], w=[("vsp", i)])
    et_rr = RR([(Et[i], ("Et", i)) for i in range(7)])
    pt_rr = RR([(Pt[i], ("Pt", i)) for i in range(7)])
    own_rr = RR(list(enumerate(vs_own)))
    pre_rr = RR(list(enumerate(vs_pre)))
    vt_slot = [-1]
    vt_bank = [None]
    for hp in range(4):
        items = []
        for ci, (window, r) in enumerate(DIL_CFG):
            nblk = TALL // r // 128
            own_n0 = nblk // 2
            if r == 1:
                quads = [[(0, n) for n in range(n0, n0 + 4)] for n0 in range(own_n0, nblk, 4)]
            elif r == 4:
                quads = [[(c, n) for n in range(own_n0, nblk)] for c in range(4)]
            else:
                quads = [[(c, 1) for c in range(c0, c0 + 4)] for c0 in range(0, 16, 4)]
            vcache = {}

            def get_vs(c, n, hp=hp, r=r, own_n0=own_n0, vcache=vcache, own_rr=own_rr, pre_rr=pre_rr):
                if (c, n) in vcache:
                    return vcache[(c, n)]
                is_pre = n < own_n0
                idx, t = (pre_rr if is_pre else own_rr).next()
                key = ("vsp" if is_pre else "vso", idx)
                tok0 = c + r * 128 * n
                vt_slot[0] = (vt_slot[0] + 1) % 4
                if vt_slot[0] == 0 or vt_bank[0] is None:
                    vt_bank[0] = ps_rr.next()
                vtp, vtk = vt_bank[0]
                sl = slice(vt_slot[0] * 128, (vt_slot[0] + 1) * 128)
                P.c("pe", "matmul", vtp[:, sl], VT[:, hp, tok0:tok0 + 127 * r + 1:r], ident[:],
                    start=True, stop=True, r=["ident"] + kv_keys("V", hp, tok0, r), w=[vtk])
                P.c("act", "copy", out=t[:, :, 0:64], in_=vtp[:, sl].rearrange("p (h d) -> p h d", h=2),
                    r=[vtk], w=[key])
                vcache[(c, n)] = (t, key)
                return t, key

            for quad in quads:
                for hl in range(2):
                    items.append((ci, r, own_n0, quad, hl, get_vs))
        st1 = {}

        def stage1(n):
            ci, r, own_n0, quad, hl, get_vs = items[n]
            vsl = [(get_vs(c, nn - 1), get_vs(c, nn)) for (c, nn) in quad]
            h = hp * 2 + hl
            pb = hl * 64
            pts = []
            for pair in range(2):
                sps, spk = ps_rr.next()
                for u in range(2):
                    c, nn = quad[pair * 2 + u]
                    tq = c + r * 128 * nn
                    tk = c + r * 128 * (nn - 1)
                    qv = QT[pb:pb + 64, hp, tq - TOWN:tq - TOWN + 127 * r + 1:r]
                    kprev = KT[pb:pb + 64, hp, tk:tk + 127 * r + 1:r]
                    kcur = KT[pb:pb + 64, hp, tq:tq + 127 * r + 1:r]
                    P.c("pe", "matmul", sps[:, u * 256:u * 256 + 128], kprev, qv, start=True, stop=True,
                        r=kv_keys("K", hp, tk, r) + kv_keys("Q", hp, tq, r), w=[spk])
                    P.c("pe", "matmul", sps[:, u * 256 + 128:u * 256 + 256], kcur, qv, start=True, stop=True,
                        r=kv_keys("K", hp, tq, r) + kv_keys("Q", hp, tq, r), w=[spk])
                et, etk = et_rr.next()
                P.c("act", "activation", out=et[:], in_=sps[:, 0:512], func=AF.Exp, scale=0.125,
                    r=[spk], w=[etk])
                pt, ptk = pt_rr.next()
                mi = ci * 8 + h
                P.c("dve", "tensor_tensor", out=pt[:], in0=et[:].rearrange("p (u k) -> p u k", u=2),
                    in1=masks[:, mi:mi + 1, :].to_broadcast([128, 2, 256]), op=ALU.mult,
                    r=[etk, "masks"], w=[ptk])
                pts.append((pt, ptk))
            st1[n] = (vsl, pts)

        def stage2(n):
            ci, r, own_n0, quad, hl, get_vs = items[n]
            vsl, pts = st1.pop(n)
            ups, upk = ps_rr.next()
            for pair in range(2):
                pt, ptk = pts[pair]
                for u in range(2):
                    qi = pair * 2 + u
                    (vp, vpk), (vc, vck) = vsl[qi]
                    P.c("pe", "matmul", ups[0:65, qi * 128:(qi + 1) * 128], vp[:, hl, :], pt[:, u, 0:128],
                        start=True, stop=False, r=[vpk, ptk], w=[upk])
                    P.c("pe", "matmul", ups[0:65, qi * 128:(qi + 1) * 128], vc[:, hl, :], pt[:, u, 128:256],
                        start=False, stop=True, r=[vck, ptk], w=[upk])
            if r == 1:
                n0 = quad[0][1] - own_n0
                dst = acc[0:65, hl, n0 * 128:n0 * 128 + 512]
                P.c("act", "copy", out=dst, in_=ups[0:65, 0:512], r=[upk], w=[("acc", hl)])
            else:
                if r == 4:
                    c = quad[0][0]
                    dst = acc[0:65, hl, c:c + 4 * 511 + 1:4]
                    src = ups[0:65, 0:512]
                else:
                    c0 = quad[0][0]
                    dst = acc[0:65, hl, :].rearrange("p (i c) -> p c i", c=16)[:, c0:c0 + 4, :]
                    src = ups[0:65, 0:512].rearrange("p (c i) -> p c i", c=4)
                P.c("dve", "tensor_tensor", out=dst, in0=dst, in1=src, op=ALU.add,
                    r=[upk, ("acc", hl)], w=[("acc", hl)])

        stage1(0)
        stage1(1)
        for n in range(len(items)):
            if n + 2 < len(items):
                stage1(n + 2)
            stage2(n)
            if n % 3 == 0:
                precast(1)
        for hl in range(2):
            h = hp * 2 + hl
            P.c("act", "activation", out=sq[:], in_=acc[0:65, hl, :], func=AF.Square,
                r=[("acc", hl)], w=["sq"])
            for ch in range(TOWN // 512):
                nps, npk = ps_rr.next()
                P.c("pe", "matmul", nps[0:64, 0:512], wn[:, :], sq[:, ch * 512:(ch + 1) * 512],
                    start=True, stop=True, r=["wn", "sq"], w=[npk])
                rs = rstd[ch % 2]
                P.c("act", "activation", out=rs[:], in_=nps[0:64, 0:512], func=AF.Ln,
                    r=[npk], w=[("rstd", ch % 2)])
                P.c("act", "activation", out=rs[:], in_=rs[:], func=AF.Exp, scale=-0.5,
                    r=[("rstd", ch % 2)], w=[("rstd", ch % 2)])
                P.c("dve", "scalar_tensor_tensor", out=o_dilT[:, h, ch * 512:(ch + 1) * 512],
                    in0=acc[0:64, hl, ch * 512:(ch + 1) * 512], scalar=gdil[:, 0:1], in1=rs[:],
                    op0=ALU.mult, op1=ALU.mult,
                    r=[("acc", hl), ("rstd", ch % 2), "gdil"], w=[("odil", h)])

    if "odil" in dbg:
        P.d("pool", "dma_start", out=odil_o, in_=o_dilT[:], r=[("odil", h) for h in range(8)], w=["odil_o"])

    P.flush()
    ph2.close()
    ph.close()

    P.skip = False
    phC = ExitStack()
    gk_all = sb("gk_all", [128, 16, 2], F32, phC)
    dest_i = sb("dest_i", [128, 32], I32, phC)
    ph = ExitStack()
    h1Ts = [sb("h1T%d" % i, [128, 8, 128], BF16, ph) for i in range(2)]
    rconst = sb("rconst", [128, 3, 128], F32, ph)
    rcb = sb("rcb", [128, 2, 128], BF16, ph)
    ohsum = sb("ohsum", [128, 32], F32, ph)
    ohsb = sb("ohsb", [128, 32], BF16, ph)
    ohb = [sb("ohb%d" % i, [128, 32], BF16, ph) for i in range(2)]
    dest_f = [sb("dest_f%d" % i, [128, 2], F32, ph) for i in range(2)]
    w_og = sb("w_og", [128, 4, 1024], BF16, ph)
    w_od = sb("w_od", [64, 8, 1024], BF16, ph)
    rw = sb("rw", [128, 8, 36], BF16, ph)
    rbias = sb("rbias", [128, 36], F32, ph)
    ln1g = sb("ln1g", [128, 1024], F32, ph)
    ln1b = sb("ln1b", [128, 1024], F32, ph)
    epsc2 = sb("epsc2", [128, 1], F32, ph)
    xt = [sb("xt%d" % i, [128, 1024], F32, ph) for i in range(2)]
    ah = [sb("ah%d" % i, [128, 1024], F32, ph) for i in range(2)]
    hpre = [sb("hpre%d" % i, [128, 1024], F32, ph) for i in range(2)]
    scr = [sb("scr%d" % i, [128, 1024], F32, ph) for i in range(2)]
    h1f = [sb("h1f%d" % i, [128, 1024], F32, ph) for i in range(2)]
    h1b = [sb("h1b%d" % i, [128, 1024], BF16, ph) for i in range(4)]
    st = [sb("st%d" % i, [128, 16], F32, ph) for i in range(2)]
    lg = [sb("lg%d" % i, [128, 36], F32, ph) for i in range(4)]
    rt = [sb("rt%d" % i, [128, 4, 32], F32, ph) for i in range(4)]
    P.d("pool", "dma_start", out=w_og[:], in_=w_og_d, w=["w_og"])
    P.d("pool", "dma_start", out=w_od[:], in_=w_od_d, w=["w_od"])
    P.d("pool", "dma_start", out=rw[:], in_=rw_d, w=["rw"])
    P.d("sp", "dma_start", out=rbias[:], in_=rbias_d, w=["rbias"])
    P.d("sp", "dma_start", out=ln1g[:], in_=ln1g_d, w=["ln1g"])
    P.d("sp", "dma_start", out=ln1b[:], in_=ln1b_d, w=["ln1b"])
    P.c("dve", "memset", epsc2[:], EPS, w=["epsc2"])
    P.d("sp", "dma_start", out=rconst[:], in_=rconst_d, w=["rconst"])
    P.c("dve", "tensor_copy", out=rcb[:], in_=rconst[:, 0:2, :], r=["rconst"], w=["rcb"])
    P.c("dve", "memset", ohsum[:], 0.0, w=["ohsum"])
    P.c("dve", "memset", ohsb[:], 0.0, w=["ohsb"])

    def layer_norm(b, src, srck, dst, dstk, gt, gk, bt, bk, stt, stk, sc, sck):
        P.c("dve", "tensor_reduce", out=stt[:, 0:1], in_=src, axis=AX.X, op=ALU.add, r=[srck], w=[stk])
        P.c("dve", "tensor_scalar", out=stt[:, 1:2], in0=stt[:, 0:1], scalar1=-1.0 / 1024, scalar2=None,
            op0=ALU.mult, r=[stk], w=[stk])
        P.c("act", "activation", out=sc, in_=src, func=AF.Identity, bias=stt[:, 1:2], scale=1.0,
            r=[srck, stk], w=[sck])
        P.c("act", "activation", out=dst, in_=sc, func=AF.Square, r=[sck], w=[dstk])
        P.c("dve", "tensor_reduce", out=stt[:, 2:3], in_=dst, axis=AX.X, op=ALU.add, r=[dstk], w=[stk])
        P.c("act", "activation", out=stt[:, 3:4], in_=stt[:, 2:3], func=AF.Ln, scale=1.0 / 1024,
            bias=epsc2[:, 0:1], r=[stk, "epsc2"], w=[stk])
        P.c("act", "activation", out=stt[:, 4:5], in_=stt[:, 3:4], func=AF.Exp, scale=-0.5, r=[stk], w=[stk])
        P.c("dve", "scalar_tensor_tensor", out=dst, in0=sc, scalar=stt[:, 4:5], in1=gt,
            op0=ALU.mult, op1=ALU.mult, r=[sck, stk, gk], w=[dstk])
        P.c("dve", "tensor_tensor", out=dst, in0=dst, in1=bt, op=ALU.add, r=[dstk, bk], w=[dstk])

    def tileX(i):
        b = i % 2
        b4 = i % 4
        h1T = h1Ts[b]
        tk = slice(i * 128, (i + 1) * 128)
        P.d("sp", "dma_start", out=xt[b][:], in_=xtok_d[tk, :], w=[("xt", b)])
        for half in range(2):
            cs_ = slice(half * 512, (half + 1) * 512)
            mps, mk = cur_rr[0].next()
            for c in range(4):
                P.c("pe", "matmul", mps[:, 0:512], o_glaT[:, c, tk], w_og[:, c, cs_], start=(c == 0), stop=False,
                    r=["w_og", ("ogla", i)], w=[mk])
            for h in range(8):
                P.c("pe", "matmul", mps[:, 0:512], o_dilT[0:64, h, tk], w_od[0:64, h, cs_], start=False,
                    stop=(h == 7), r=["w_od", ("odil", h)], w=[mk])
            P.c("dve", "scalar_tensor_tensor", out=hpre[b][:, cs_], in0=xt[b][:, cs_], scalar=ALPHA,
                in1=mps[:, 0:512], op0=ALU.mult, op1=ALU.add, r=[("xt", b), mk], w=[("hpre", b)])
        layer_norm(b, hpre[b][:], ("hpre", b), h1f[b][:], ("h1f", b), ln1g[:], "ln1g", ln1b[:], "ln1b",
                   st[b], ("st", b), scr[b][:], ("scr", b))
        P.c("act", "mul", out=ah[b][:], in_=h1f[b][:], mul=ALPHA, r=[("h1f", b)], w=[("ah", b)])
        P.d("sp", "dma_start", out=h1buf[tk, :], in_=ah[b][:], r=[("ah", b)], w=[("h1buf", i)])
        P.c("dve", "tensor_copy", out=h1b[b4][:], in_=h1f[b][:], r=[("h1f", b)], w=[("h1b", b4)])
        tps_, tpk_ = cur_rr[0].next()
        tpb_ = tps_[:].bitcast(BF16)
        for dc in range(8):
            P.c("pe", "transpose", out=tpb_[:, dc * 128:(dc + 1) * 128], in_=h1b[b4][:, dc * 128:(dc + 1) * 128],
                identity=ident[:], r=["ident", ("h1b", b4)], w=[tpk_])
        P.c("act", "copy", out=h1T[:, :, :], in_=tpb_[:, 0:1024].rearrange("p (c t) -> p c t", c=8),
            r=[tpk_], w=[("h1T", b)])
        lps, lk = cur_rr[0].next()
        for dc in range(8):
            P.c("pe", "matmul", lps[:, 0:36], h1T[:, dc, :], rw[:, dc, :], start=(dc == 0), stop=(dc == 7),
                r=["rw", ("h1T", b)], w=[lk])
        L = lg[b4]
        Lk = ("lg", b4)
        S = st[b]
        Sk = ("st", b)
        R_ = rt[b4]
        Rk = ("rt", b4)
        P.c("dve", "tensor_tensor", out=L[:], in0=lps[:, 0:36], in1=rbias[:], op=ALU.add, r=[lk, "rbias"], w=[Lk])
        P.c("dve", "tensor_reduce", out=S[:, 5:6], in_=L[:, 0:4], axis=AX.X, op=ALU.max, r=[Lk], w=[Sk])
        P.c("dve", "tensor_scalar", out=S[:, 6:7], in0=S[:, 5:6], scalar1=-1.0, scalar2=None, op0=ALU.mult,
            r=[Sk], w=[Sk])
        P.c("act", "activation", out=R_[:, 0, 0:4], in_=L[:, 0:4], func=AF.Exp, bias=S[:, 6:7], scale=1.0,
            r=[Lk, Sk], w=[Rk])
        P.c("dve", "tensor_reduce", out=S[:, 7:8], in_=R_[:, 0, 0:4], axis=AX.X, op=ALU.add, r=[Rk], w=[Sk])
        P.c("dve", "reciprocal", out=S[:, 7:8], in_=S[:, 7:8], r=[Sk], w=[Sk])
        P.c("dve", "tensor_scalar", out=R_[:, 0, 4:8], in0=L[:, 0:4], scalar1=S[:, 5:6], scalar2=None,
            op0=ALU.is_ge, r=[Lk, Sk], w=[Rk])
        P.c("dve", "tensor_scalar", out=R_[:, 0, 8:12], in0=R_[:, 0, 4:8], scalar1=1e9, scalar2=-1e9,
            op0=ALU.mult, op1=ALU.add, r=[Rk], w=[Rk])
        P.c("dve", "tensor_tensor", out=R_[:, 1, :].rearrange("p (g e) -> p g e", g=4),
            in0=L[:, 4:36].rearrange("p (g e) -> p g e", g=4),
            in1=R_[:, 0, 8:12].unsqueeze(2).to_broadcast([128, 4, 8]), op=ALU.add, r=[Lk, Rk], w=[Rk])
        P.c("dve", "tensor_reduce", out=S[:, 8:9], in_=R_[:, 1, :], axis=AX.X, op=ALU.max, r=[Rk], w=[Sk])
        P.c("dve", "tensor_scalar", out=R_[:, 2, :], in0=R_[:, 1, :], scalar1=S[:, 8:9], scalar2=None,
            op0=ALU.is_ge, r=[Rk, Sk], w=[Rk])
        P.c("dve", "scalar_tensor_tensor", out=R_[:, 1, :], in0=R_[:, 2, :], scalar=-1e9, in1=R_[:, 1, :],
            op0=ALU.mult, op1=ALU.add, r=[Rk], w=[Rk])
        P.c("dve", "tensor_reduce", out=S[:, 9:10], in_=R_[:, 1, :], axis=AX.X, op=ALU.max, r=[Rk], w=[Sk])
        P.c("dve", "tensor_scalar", out=R_[:, 3, :], in0=R_[:, 1, :], scalar1=S[:, 9:10], scalar2=None,
            op0=ALU.is_ge, r=[Rk, Sk], w=[Rk])
        P.c("dve", "tensor_tensor", out=S[:, 10:11], in0=S[:, 9:10], in1=S[:, 8:9], op=ALU.subtract,
            r=[Sk], w=[Sk])
        P.c("act", "activation", out=S[:, 11:12], in_=S[:, 10:11], func=AF.Exp, r=[Sk], w=[Sk])
        P.c("dve", "tensor_scalar", out=S[:, 12:13], in0=S[:, 11:12], scalar1=1.0, scalar2=None, op0=ALU.add,
            r=[Sk], w=[Sk])
        P.c("dve", "reciprocal", out=S[:, 12:13], in_=S[:, 12:13], r=[Sk], w=[Sk])
        P.c("dve", "tensor_tensor", out=S[:, 13:14], in0=S[:, 11:12], in1=S[:, 12:13], op=ALU.mult,
            r=[Sk], w=[Sk])
        P.c("dve", "tensor_scalar", out=S[:, 12:14], in0=S[:, 12:14], scalar1=S[:, 7:8], scalar2=None,
            op0=ALU.mult, r=[Sk], w=[Sk])
        P.c("dve", "tensor_copy", out=gk_all[:, i, :], in_=S[:, 12:14], r=[Sk], w=[("gk", i)])
    def tileY(i):
        b = i % 2
        b4 = i % 4
        L = lg[b4]
        Lk = ("lg", b4)
        S = st[b]
        Sk = ("st", b)
        R_ = rt[b4]
        Rk = ("rt", b4)
        ob = ohb[b]
        obk = ("ohb", b)
        P.c("dve", "tensor_tensor", out=R_[:, 0, :], in0=R_[:, 2, :], in1=R_[:, 3, :], op=ALU.add, r=[Rk], w=[Rk])
        P.c("dve", "tensor_copy", out=ob[:], in_=R_[:, 0, :], r=[Rk], w=[obk])
        rps, rk_ = cur_rr[0].next()
        P.c("pe", "matmul", rps[:, 0:32], rcb[:, 0, :], ob[:], start=True, stop=False, r=["rcb", obk], w=[rk_])
        P.c("pe", "matmul", rps[:, 0:32], rcb[:, 1, :], ohsb[:], start=False, stop=True, r=["rcb", "ohsb"], w=[rk_])
        P.c("dve", "tensor_copy", out=R_[:, 1, :], in_=rps[:, 0:32], r=[rk_], w=[Rk])
        P.c("dve", "tensor_scalar", out=L[:, 0:32], in0=R_[:, 1, :], scalar1=float(CAP), scalar2=1e6,
            op0=ALU.is_ge, op1=ALU.mult, r=[Rk], w=[Lk])
        P.c("dve", "tensor_tensor", out=R_[:, 1, :], in0=R_[:, 1, :], in1=L[:, 0:32], op=ALU.add, r=[Rk, Lk], w=[Rk])
        P.c("dve", "tensor_tensor", out=R_[:, 1, :], in0=R_[:, 1, :], in1=rconst[:, 2, 0:32], op=ALU.add,
            r=[Rk, "rconst"], w=[Rk])
        df = dest_f[b]
        dfk = ("dest_f", b)
        for k_ in range(2):
            P.c("dve", "tensor_tensor", out=L[:, 0:32], in0=R_[:, 2 + k_, :], in1=R_[:, 1, :], op=ALU.mult,
                r=[Rk, Lk], w=[Lk])
            P.c("dve", "tensor_reduce", out=df[:, k_:k_ + 1], in_=L[:, 0:32], axis=AX.X, op=ALU.add,
                r=[Lk], w=[dfk])
        P.c("dve", "tensor_copy", out=dest_i[:, 2 * i:2 * i + 2], in_=df[:], r=[dfk], w=[("dest", i)])
        P.c("dve", "tensor_tensor", out=ohsum[:], in0=ohsum[:], in1=R_[:, 0, :], op=ALU.add, r=["ohsum", Rk], w=["ohsum"])
        P.c("dve", "tensor_copy", out=ohsb[:], in_=ohsum[:], r=["ohsum"], w=["ohsb"])
        for k_ in range(2):
            P.d("pool", "indirect_dma_start", out=xbuf[:, :],
                out_offset=bass.IndirectOffsetOnAxis(ap=dest_i[:, 2 * i + k_:2 * i + k_ + 1], axis=0),
                in_=h1b[b4][:, :], in_offset=None, bounds_check=bc_reg, oob_is_err=False,
                r=[("h1b", b4), ("dest", i)], w=[("xbuf", i, k_)])
    for i in range(0, 16, 2):
        streams = []
        for ii, rr_ in ((i, rrA), (i + 1, rrB)):
            cur_rr[0] = rr_
            P.capture()
            tileX(ii)
            streams.append(P.end_capture())
        if i > 0:
            cur_rr[0] = rrG
            P.capture()
            tileY(i - 2)
            tileY(i - 1)
            streams.append(P.end_capture())
        P.replay_interleaved(streams)
        precast(2)
    cur_rr[0] = ps_rr
    precast(3 * NEXP)
    tileY(14)
    tileY(15)
    P.flush()
    ph.close()

    ph = ExitStack()
    wg = [sb("wg%d" % i, [128, 8, 512], BF16, ph) for i in range(3)]
    wu = [sb("wu%d" % i, [128, 8, 512], BF16, ph) for i in range(3)]
    wd = [sb("wd%d" % i, [128, 4, 1024], BF16, ph) for i in range(3)]
    xg = [sb("xg%d" % i, [128, 2, 1024], BF16, ph) for i in range(2)]
    XT = [sb("XT%d" % i, [128, 8, CAP], BF16, ph) for i in range(2)]
    hidT = [sb("hidT%d" % i, [128, 4, CAP], BF16, ph) for i in range(2)]
    sg = [sb("sg%d" % i, [128, CAP], F32, ph) for i in range(2)]
    ysb = [sb("ysb%d" % i, [128, 2, 1024], BF16, ph) for i in range(2)]
    sgi = 0
    evi = 0

    def loads_x(e):
        wb = e % 2
        P.d("pool", "dma_start", out=xg[wb][:], in_=xbuf[e * CAP:(e + 1) * CAP, :].rearrange("(k p) d -> p k d", p=128),
            w=[("xg", wb)])

    def loads_w(e):
        w3 = e % 3
        P.d("sp", "dma_start", out=wg[w3][:].rearrange("p c f -> p (c f)"), in_=wgb[e], w=[("wg", w3)])
        P.d("sp", "dma_start", out=wu[w3][:].rearrange("p c f -> p (c f)"), in_=wub[e], w=[("wu", w3)])
        P.d("sp", "dma_start", out=wd[w3][:].rearrange("p c f -> p (c f)"), in_=wdb[e], w=[("wd", w3)])

    def prep(e):
        nonlocal evi
        wb = e % 2
        for blk in range(2):
            for hq in range(2):
                tps_, tpk_ = ps_rr.next()
                for d4 in range(4):
                    dc = hq * 4 + d4
                    P.c("pe", "matmul", tps_[:, d4 * 128:(d4 + 1) * 128], xg[wb][:, blk, dc * 128:(dc + 1) * 128],
                        ident[:], start=True, stop=True, r=["ident", ("xg", wb)], w=[tpk_])
                evi += 1
                if evi % 2:
                    P.c("act", "copy", out=XT[wb][:, hq * 4:hq * 4 + 4, blk * 128:(blk + 1) * 128],
                        in_=tps_[:, 0:512].rearrange("p (c t) -> p c t", c=4), r=[tpk_], w=[("XT", wb)])
                else:
                    P.c("dve", "tensor_copy", out=XT[wb][:, hq * 4:hq * 4 + 4, blk * 128:(blk + 1) * 128],
                        in_=tps_[:, 0:512].rearrange("p (c t) -> p c t", c=4), r=[tpk_], w=[("XT", wb)])

    loads_x(0)
    loads_w(0)
    loads_w(1)
    prep(0)
    for e in range(NEXP):
        wb = e % 2
        w3 = e % 3
        if e + 1 < NEXP:
            loads_x(e + 1)
        if e + 2 < NEXP:
            loads_w(e + 2)
        for ft in range(4):
            fs = slice(ft * 128, (ft + 1) * 128)
            gps, gk = ps_rr.next()
            ups2, uk2 = ps_rr.next()
            for dc in range(8):
                P.c("pe", "matmul", gps[:, 0:CAP], wg[w3][:, dc, fs], XT[wb][:, dc, :],
                    start=(dc == 0), stop=(dc == 7), r=[("wg", w3), ("XT", wb)], w=[gk])
            s_ = sg[sgi % 2]
            sk_ = ("sg", sgi % 2)
            sgi += 1
            P.c("act", "activation", out=s_[:], in_=gps[:, 0:CAP], func=AF.Silu, r=[gk], w=[sk_])
            for dc in range(8):
                P.c("pe", "matmul", ups2[:, 0:CAP], wu[w3][:, dc, fs], XT[wb][:, dc, :],
                    start=(dc == 0), stop=(dc == 7), r=[("wu", w3), ("XT", wb)], w=[uk2])
            P.c("dve", "tensor_tensor", out=hidT[wb][:, ft, :], in0=ups2[:, 0:CAP], in1=s_[:], op=ALU.mult,
                r=[uk2, sk_], w=[("hidT", wb)])
        if e + 1 < NEXP:
            prep(e + 1)
        for blk in range(2):
            for half in range(2):
                cs_ = slice(half * 512, (half + 1) * 512)
                yps, yk = ps_rr.next()
                for fc in range(4):
                    P.c("pe", "matmul", yps[:, 0:512], hidT[wb][:, fc, blk * 128:(blk + 1) * 128],
                        wd[w3][:, fc, cs_], start=(fc == 0), stop=(fc == 3),
                        r=[("hidT", wb), ("wd", w3)], w=[yk])
                evi += 1
                if evi % 2:
                    P.c("act", "copy", out=ysb[wb][:, blk, cs_], in_=yps[:, 0:512], r=[yk], w=[("ysb", wb)])
                else:
                    P.c("dve", "tensor_copy", out=ysb[wb][:, blk, cs_], in_=yps[:, 0:512], r=[yk], w=[("ysb", wb)])
        P.d("pool", "dma_start", out=ybuf[e * CAP:(e + 1) * CAP, :].rearrange("(k p) d -> p k d", p=128),
            in_=ysb[wb][:], r=[("ysb", wb)], w=[("ybuf", e)])
    P.flush()
    ph.close()

    ph = ExitStack()
    ln2g = sb("ln2g", [128, 1024], F32, ph)
    ln2b = sb("ln2b", [128, 1024], F32, ph)
    epsc2 = sb("epsc2b", [128, 1], F32, ph)
    scr = [sb("scrE%d" % i, [128, 1024], F32, ph) for i in range(4)]
    ot = [sb("ot%d" % i, [128, 1024], F32, ph) for i in range(4)]
    st = [sb("stE%d" % i, [128, 16], F32, ph) for i in range(4)]
    yg = [sb("yg%d" % i, [128, 2, 1024], BF16, ph) for i in range(4)]
    ac = [sb("ac%d" % i, [128, 1024], F32, ph) for i in range(4)]
    P.d("sp", "dma_start", out=ln2g[:], in_=ln2g_d, w=["ln2g"])
    P.d("sp", "dma_start", out=ln2b[:], in_=ln2b_d, w=["ln2b"])
    P.c("dve", "memset", epsc2[:], EPS, w=["epsc2"])
    def tileE(i):
        b = i % 4
        y_ = yg[i % 4]
        yk_ = ("yg", i % 4)
        a_ = ac[i % 4]
        ak_ = ("ac", i % 4)
        P.d("sp", "dma_start", out=a_[:], in_=h1buf[i * 128:(i + 1) * 128, :], w=[ak_])
        P.c("pool", "memset", y_[:], 0.0, w=[yk_])
        for k_ in range(2):
            P.d("pool", "indirect_dma_start", out=y_[:, k_, :], out_offset=None, in_=ybuf[:, :],
                in_offset=bass.IndirectOffsetOnAxis(ap=dest_i[:, 2 * i + k_:2 * i + k_ + 1], axis=0),
                bounds_check=bc_reg, oob_is_err=False, r=[yk_], w=[yk_])
        for k_ in range(2):
            P.c("dve", "scalar_tensor_tensor", out=a_[:], in0=y_[:, k_, :], scalar=gk_all[:, i, k_:k_ + 1],
                in1=a_[:], op0=ALU.mult, op1=ALU.add, r=[yk_, ak_], w=[ak_])
        layer_norm(b, a_[:], ak_, ot[b][:], ("ot", b), ln2g[:], "ln2g", ln2b[:], "ln2b",
                   st[b], ("stE", b), scr[b][:], ("scrE", b))
        P.d("sp", "dma_start", out=out_d[i * 128:(i + 1) * 128, :], in_=ot[b][:], r=[("ot", b)], w=[("out", i)])

    for i in range(0, 16, 4):
        streams = []
        for ii in range(i, i + 4):
            P.capture()
            tileE(ii)
            streams.append(P.end_capture())
        P.replay_interleaved(streams)
    P.flush()
    ph.close()
    phC.close()

    P.skip = False
    stats = P.stats
    return nc, es, stats


def host_consts():
    j = np.arange(128)[:, None].astype(np.float64)
    i = np.arange(128)[None, :].astype(np.float64)
    masks = np.zeros((128, 24, 256), np.float32)
    for ci, (w, r) in enumerate(DIL_CFG):
        for h in range(8):
            s = 2.0 ** (-(h + 1))
            rel_p = i + 128 - j
            rel_c = i - j
            masks[:, ci * 8 + h, 0:128] = np.where(i <= j, np.exp(-s * r * rel_p), 0.0)
            masks[:, ci * 8 + h, 128:256] = np.where(i >= j, np.exp(-s * r * np.maximum(rel_c, 0)), 0.0)
    wn = np.full((65, 64), 1.0 / 64, np.float32)
    wn[64, :] = EPS
    sI = np.arange(128)[:, None]
    tI = np.arange(128)[None, :]
    cmat = np.zeros((128, 4, 128), np.float32)
    cmat[:, 0, :] = np.where(sI <= tI, -1.0 / 16, 0.0)
    cmat[:, 1, :] = np.where(sI > tI, -1.0 / 16, 0.0)
    cmat[:, 2, :] = np.where(sI <= tI, 1.0, 0.0)
    cmat[:, 3, :] = -1.0 / 16
    rconst = np.zeros((128, 3, 128), np.float32)
    rconst[:, 0, :] = np.where(sI < tI, 1.0, 0.0)
    rconst[:, 1, :] = 1.0
    rconst[:, 2, 0:32] = (np.arange(32) * CAP)[None, :]
    return dict(masks=masks, ident=np.eye(128, dtype=np.float32), wn=wn, cmat=cmat, rconst=rconst)


def make_in_maps(inputs):
    x = np.asarray(inputs["x"], np.float32)
    w_in = np.asarray(inputs["w_in"], np.float32)[0]
    cs = host_consts()
    w_inA = np.concatenate([w_in[:, 1552:2064], w_in[:, 2064:2576], w_in[:, 2576:3088]], axis=1)
    w_inA = np.ascontiguousarray(w_inA.reshape(8, 128, 1536).transpose(1, 0, 2))
    gdil = np.asarray(inputs["dil_norm_g"], np.float32)[0].reshape(64, 1)
    w_inB = np.ascontiguousarray(w_in[:, 0:1552].reshape(8, 128, 1552).transpose(1, 0, 2))
    w2aug = np.concatenate([np.asarray(inputs["gla_gate_w2"], np.float32)[0],
                            np.asarray(inputs["gla_gate_b"], np.float32)[0][None, :]], axis=0)
    gnorm = np.ascontiguousarray(np.broadcast_to(np.asarray(inputs["gla_norm_g"], np.float32)[0][None, :], (128, 128)))
    w_out = np.asarray(inputs["w_out"], np.float32)[0]
    w_og = np.ascontiguousarray(w_out[0:512].reshape(4, 128, 1024).transpose(1, 0, 2))
    w_od = np.ascontiguousarray(w_out[512:1024].reshape(8, 64, 1024).transpose(1, 0, 2))
    rwm = np.concatenate([np.asarray(inputs["router_coarse_w"], np.float32)[0],
                          np.asarray(inputs["router_fine_w"], np.float32)[0].reshape(1024, 32)], axis=1)
    rwm = np.ascontiguousarray(rwm.reshape(8, 128, 36).transpose(1, 0, 2))
    rb = np.concatenate([np.asarray(inputs["router_coarse_b"], np.float32)[0],
                         np.asarray(inputs["router_fine_b"], np.float32)[0].reshape(32)])
    rbias = np.ascontiguousarray(np.broadcast_to(rb[None, :], (128, 36)))

    def bc(name):
        return np.ascontiguousarray(np.broadcast_to(np.asarray(inputs[name], np.float32)[0][None, :], (128, 1024)))

    wgm = np.ascontiguousarray(np.asarray(inputs["expert_w_gate"], np.float32)[0].reshape(NEXP, 8, 128, 512).transpose(0, 2, 1, 3))
    wum = np.ascontiguousarray(np.asarray(inputs["expert_w_up"], np.float32)[0].reshape(NEXP, 8, 128, 512).transpose(0, 2, 1, 3))
    wdm = np.ascontiguousarray(np.asarray(inputs["expert_w_down"], np.float32)[0].reshape(NEXP, 4, 128, 1024).transpose(0, 2, 1, 3))
    shared = dict(w_og=w_og, w_od=w_od, rw=rwm, rbias=rbias, ln1g=bc("ln1_g"), ln1b=bc("ln1_b"),
                  ln2g=bc("ln2_g"), ln2b=bc("ln2_b"), wg=wgm, wu=wum, wd=wdm, rconst=cs["rconst"])
    maps = []
    for c in range(NCORES):
        b, half = c // 2, c % 2
        xT = np.zeros((1024, TALL), np.float32)
        if half == 1:
            xT[:, :TOWN] = x[b, :TOWN].T
        xT[:, TOWN:] = x[b, half * TOWN:(half + 1) * TOWN].T
        maps.append(dict(xT=xT, w_inA=w_inA, masks=cs["masks"], ident=cs["ident"], wn=cs["wn"],
                         gdil=gdil, pv=np.full((128, 1), float(half), np.float32),
                         w_inB=w_inB, w2aug=w2aug, gnorm=gnorm, cmat=cs["cmat"],
                         xtok=np.ascontiguousarray(x[b, half * TOWN:(half + 1) * TOWN]), **shared))
    return maps


def kernel(**inputs):
    nc, es, stats = build_program()
    with es:
        res = run_bass_kernel_spmd(nc, make_in_maps(inputs), core_ids=list(range(NCORES)))
    out = np.zeros((4, 2 * TOWN, 1024), np.float32)
    for c in range(NCORES):
        out[c // 2, (c % 2) * TOWN:(c % 2 + 1) * TOWN] = res.results[c]["out"]
    return out
```
